# Optimizing a Trainium2 kernel written in Bass

```python
import jax, jax.numpy as jnp
from jax import lax
import numpy as np

D_MODEL = 1024
BATCH = 4
SEQ = 4096
DEPTH = 2

MEM_LEN = 256
N_EVEN = (DEPTH + 1) // 2
N_ODD = DEPTH // 2

MIX_WIDTH = D_MODEL
A_WIDTH = MIX_WIDTH // 2
A_CONV = 3
B_WIDTH = MIX_WIDTH - A_WIDTH
POOL_WINDOWS = (2, 4, 8, 16)
B_GROUP = B_WIDTH // len(POOL_WINDOWS)
IN_COLS = 3 * A_WIDTH + B_WIDTH

C_CONV = 31

XATTN_HEADS = 4
XATTN_HEAD_DIM = D_MODEL // XATTN_HEADS

D_FF = ((8 * D_MODEL // 3 + 255) // 256) * 256
N_EXPERTS = 8
TOP_K = 2
D_FF_EXPERT = 7 * D_MODEL // 2

EPS = 1e-6

kernel_name = "hybrid_conv_pool_conformer_moe_encoder"


def rmsnorm(x, g):
    xf = x.astype(jnp.float32)
    y = xf * lax.rsqrt(jnp.mean(xf * xf, axis=-1, keepdims=True) + EPS)
    return (y * g.astype(jnp.float32)).astype(x.dtype)


def layernorm(x, g, b):
    xf = x.astype(jnp.float32)
    mu = jnp.mean(xf, axis=-1, keepdims=True)
    xc = xf - mu
    var = jnp.mean(xc * xc, axis=-1, keepdims=True)
    y = xc * lax.rsqrt(var + EPS) * g.astype(jnp.float32) + b.astype(jnp.float32)
    return y.astype(x.dtype)


def depthwise_conv(x, w):
    k = w.shape[0]
    c = x.shape[-1]
    return lax.conv_general_dilated(
        x, w[:, None, :].astype(x.dtype), window_strides=(1,),
        padding=[(k // 2, k // 2)], dimension_numbers=("NWC", "WIO", "NWC"),
        feature_group_count=c)


def centred_window_mean(u, window):
    s = u.shape[1]
    left = window // 2
    right = window - 1 - left
    t = np.arange(s)
    lo = np.maximum(t - left, 0)
    hi = np.minimum(t + right, s - 1) + 1
    uf = u.astype(jnp.float32)
    cs = jnp.concatenate([jnp.zeros_like(uf[:, :1]), jnp.cumsum(uf, axis=1)], axis=1)
    count = jnp.asarray((hi - lo).astype(np.float32))[None, :, None]
    return ((cs[:, hi] - cs[:, lo]) / count).astype(u.dtype)


def short_conv_mixer(h, gate_b, gate_c, conv_w):
    return gate_b * depthwise_conv(gate_c * h, conv_w)


def pool_mixer(u, w_groups, scale):
    outs = []
    for g, win in enumerate(POOL_WINDOWS):
        ug = u[..., g * B_GROUP:(g + 1) * B_GROUP]
        outs.append(centred_window_mean(ug, win) - ug)
    p = jnp.stack(outs, axis=2)
    y = jnp.einsum("bsgc,gcd->bsgd", p, w_groups)
    return y.reshape(u.shape) * scale


def even_mixer(xn, w_in, conv_a, pool_w, pool_scale, w_out):
    z = xn @ w_in
    h, gb, gc, u = jnp.split(z, [A_WIDTH, 2 * A_WIDTH, 3 * A_WIDTH], axis=-1)
    y = jnp.concatenate([short_conv_mixer(h, gb, gc, conv_a),
                         pool_mixer(u, pool_w, pool_scale)], axis=-1)
    return y @ w_out


def conformer_conv(xn, pw1_w, pw1_b, dw_w, dw_b, ln_g, ln_b, pw2_w, pw2_b):
    a, g = jnp.split(xn @ pw1_w + pw1_b, 2, axis=-1)
    h = a * jax.nn.sigmoid(g)
    h = depthwise_conv(h, dw_w) + dw_b
    h = jax.nn.silu(layernorm(h, ln_g, ln_b))
    return h @ pw2_w + pw2_b


def cross_attn(xn, mem, norm_mem, wq, wkv, wo):
    b, s, d = xn.shape
    m = mem.shape[1]
    q = (xn @ wq).reshape(b, s, XATTN_HEADS, XATTN_HEAD_DIM)
    k, v = jnp.split(rmsnorm(mem, norm_mem) @ wkv, 2, axis=-1)
    k = k.reshape(b, m, XATTN_HEADS, XATTN_HEAD_DIM)
    v = v.reshape(b, m, XATTN_HEADS, XATTN_HEAD_DIM)
    sc = jnp.einsum("bshd,bmhd->bhsm", q, k).astype(jnp.float32) * (XATTN_HEAD_DIM ** -0.5)
    p = jax.nn.softmax(sc, axis=-1).astype(v.dtype)
    o = jnp.einsum("bhsm,bmhd->bshd", p, v).reshape(b, s, d)
    return o @ wo


def swiglu(xn, w_gu, w_down):
    g, u = jnp.split(xn @ w_gu, 2, axis=-1)
    return (jax.nn.silu(g) * u) @ w_down


def moe_swiglu(xn, router, w_gu, w_down):
    logits = (xn @ router).astype(jnp.float32)
    top_v, top_i = lax.top_k(logits, TOP_K)
    w = jax.nn.softmax(top_v, axis=-1)
    comb = jnp.sum(jax.nn.one_hot(top_i, N_EXPERTS, dtype=jnp.float32) * w[..., None],
                   axis=-2).astype(xn.dtype)
    out = jnp.zeros_like(xn)
    for e in range(N_EXPERTS):
        out = out + comb[..., e:e + 1] * swiglu(xn, w_gu[e], w_down[e])
    return out


def setup_inputs(seed: int = 0) -> dict:
    key = jax.random.key(seed)
    ks = iter(jax.random.split(key, 40))
    f32 = jnp.float32

    def nrm(shape, scale):
        return jax.random.normal(next(ks), shape, f32) * scale

    def gain(shape):
        return 1.0 + 0.02 * jax.random.normal(next(ks), shape, f32)

    D = D_MODEL
    return {
        "x": nrm((BATCH, SEQ, D), 1.0),
        "mem": nrm((BATCH, MEM_LEN, D), 1.0),
        "ev_norm_mix": gain((N_EVEN, D)),
        "ev_w_in": nrm((N_EVEN, D, IN_COLS), D ** -0.5),
        "ev_conv_a": nrm((N_EVEN, A_CONV, A_WIDTH), A_CONV ** -0.5),
        "ev_pool_w": nrm((N_EVEN, len(POOL_WINDOWS), B_GROUP, B_GROUP), B_GROUP ** -0.5),
        "ev_pool_scale": gain((N_EVEN, B_WIDTH)),
        "ev_w_out": nrm((N_EVEN, MIX_WIDTH, D), MIX_WIDTH ** -0.5),
        "ev_norm_ffn": gain((N_EVEN, D)),
        "ev_ffn_gu": nrm((N_EVEN, D, 2 * D_FF), D ** -0.5),
        "ev_ffn_down": nrm((N_EVEN, D_FF, D), D_FF ** -0.5),
        "od_norm_mix": gain((N_ODD, D)),
        "od_pw1_w": nrm((N_ODD, D, 2 * D), D ** -0.5),
        "od_pw1_b": nrm((N_ODD, 2 * D), 0.02),
        "od_dw_w": nrm((N_ODD, C_CONV, D), C_CONV ** -0.5),
        "od_dw_b": nrm((N_ODD, D), 0.02),
        "od_ln_g": gain((N_ODD, D)),
        "od_ln_b": nrm((N_ODD, D), 0.02),
        "od_pw2_w": nrm((N_ODD, D, D), D ** -0.5),
        "od_pw2_b": nrm((N_ODD, D), 0.02),
        "od_norm_moe": gain((N_ODD, D)),
        "od_router": nrm((N_ODD, D, N_EXPERTS), D ** -0.5),
        "od_moe_gu": nrm((N_ODD, N_EXPERTS, D, 2 * D_FF_EXPERT), D ** -0.5),
        "od_moe_down": nrm((N_ODD, N_EXPERTS, D_FF_EXPERT, D), D_FF_EXPERT ** -0.5),
        "xa_norm": gain((DEPTH, D)),
        "xa_norm_mem": gain((DEPTH, D)),
        "xa_wq": nrm((DEPTH, D, D), D ** -0.5),
        "xa_wkv": nrm((DEPTH, D, 2 * D), D ** -0.5),
        "xa_wo": nrm((DEPTH, D, D), D ** -0.5),
        "final_norm": gain((D,)),
    }


def reference(x, mem,
              ev_norm_mix, ev_w_in, ev_conv_a, ev_pool_w, ev_pool_scale, ev_w_out,
              ev_norm_ffn, ev_ffn_gu, ev_ffn_down,
              od_norm_mix, od_pw1_w, od_pw1_b, od_dw_w, od_dw_b, od_ln_g, od_ln_b,
              od_pw2_w, od_pw2_b, od_norm_moe, od_router, od_moe_gu, od_moe_down,
              xa_norm, xa_norm_mem, xa_wq, xa_wkv, xa_wo, final_norm):
    h = x
    for layer in range(DEPTH):
        i = layer // 2
        if layer % 2 == 0:
            h = h + even_mixer(rmsnorm(h, ev_norm_mix[i]), ev_w_in[i], ev_conv_a[i],
                               ev_pool_w[i], ev_pool_scale[i], ev_w_out[i])
        else:
            h = h + conformer_conv(rmsnorm(h, od_norm_mix[i]), od_pw1_w[i], od_pw1_b[i],
                                   od_dw_w[i], od_dw_b[i], od_ln_g[i], od_ln_b[i],
                                   od_pw2_w[i], od_pw2_b[i])
        h = h + cross_attn(rmsnorm(h, xa_norm[layer]), mem, xa_norm_mem[layer],
                           xa_wq[layer], xa_wkv[layer], xa_wo[layer])
        if layer % 2 == 0:
            h = h + swiglu(rmsnorm(h, ev_norm_ffn[i]), ev_ffn_gu[i], ev_ffn_down[i])
        else:
            h = h + moe_swiglu(rmsnorm(h, od_norm_moe[i]), od_router[i],
                               od_moe_gu[i], od_moe_down[i])
    return rmsnorm(h, final_norm)
```

```python
from contextlib import ExitStack
import numpy as np
import concourse.bass as bass
import concourse.mybir as mybir
from concourse.bass_utils import run_bass_kernel_spmd

F32 = mybir.dt.float32
BF16 = mybir.dt.bfloat16
AF = mybir.ActivationFunctionType
ALU = mybir.AluOpType
AX = mybir.AxisListType

PE, ACT, DVE, POOL, SP = "tensor", "scalar", "vector", "gpsimd", "sync"
ENGINES = (PE, ACT, DVE, POOL, SP)

D = 1024
NCH = 8
SEQ = 4096
OWN = 2048
HALO = 24
N0 = OWN + 2 * HALO
OWN0 = HALO
R1LO, R1HI = 8, 2088
NR1 = R1HI - R1LO
T0 = [(R1LO + 416 * i, R1LO + 416 * (i + 1)) for i in range(5)]
T1 = [(OWN0 + 512 * i, OWN0 + 512 * (i + 1)) for i in range(4)]
TC = [(OWN0 + 410 * i, min(OWN0 + 410 * (i + 1), OWN0 + OWN)) for i in range(5)]
TN0 = [(0, 420), (420, 840), (840, 1260), (1260, 1680), (1680, 2096)]
MEM = 256
DFF = 2816
DFFE = 3584
NEXP = 8
EPS = 1e-6

V_EV_NORM_MIX, V_EV_NORM_FFN, V_XA_NORM0, V_XA_NORMMEM0, V_XA_NORM1, V_XA_NORMMEM1 = 0, 1, 2, 3, 4, 5
V_OD_NORM_MIX, V_PW1_BA, V_PW1_BG, V_DW_B, V_LN_G, V_LN_B, V_PW2_B, V_NORM_MOE, V_FINAL = 6, 7, 8, 9, 10, 11, 12, 13, 14
V_POOL_SCALE = 15
V_CONVA = 16
V_DW = 19
NVEC = 50


class Op:
    __slots__ = ("eng", "fn", "deps", "dmakey", "dmaval", "signal", "val", "idx", "dmawaits")

    def __init__(self, eng, fn, dmakey, idx):
        self.eng = eng
        self.fn = fn
        self.deps = {}
        self.dmawaits = {}
        self.dmakey = dmakey
        self.dmaval = 0
        self.signal = False
        self.val = 0
        self.idx = idx


class Prog:
    def __init__(self, dry=False):
        self.ops = {e: [] for e in ENGINES}
        self.bufs = {}
        self.dma_count = {}
        self.n = 0
        self.dry = dry

    def add(self, eng, fn, reads=(), writes=(), dma=None):
        if self.dry:
            return None
        op = Op(eng, fn, dma, self.n)
        self.n += 1
        deps = []
        for (name, lo, hi) in reads:
            b = self.bufs.get(name)
            if b is None:
                b = self.bufs[name] = ([], [])
            for (l, h, o) in b[0]:
                if l < hi and lo < h:
                    deps.append(o)
        for (name, lo, hi) in writes:
            b = self.bufs.get(name)
            if b is None:
                b = self.bufs[name] = ([], [])
            for (l, h, o) in b[0]:
                if l < hi and lo < h:
                    deps.append(o)
            for (l, h, o) in b[1]:
                if l < hi and lo < h:
                    deps.append(o)
        for o in deps:
            if o.dmakey is not None:
                v = 16 * self.dma_count[o.dmakey]
                if op.dmawaits.get(o.dmakey, 0) < v:
                    op.dmawaits[o.dmakey] = v
            else:
                if o.eng == PE and eng == PE:
                    continue
                cur = op.deps.get(o.eng)
                if cur is None or cur.idx < o.idx:
                    op.deps[o.eng] = o
        for o in op.deps.values():
            o.signal = True
        isdma = dma is not None
        for (name, lo, hi) in reads:
            b = self.bufs[name]
            if not isdma:
                b[1][:] = [(l, h, o) for (l, h, o) in b[1]
                           if not (o.eng == eng and o.dmakey is None and lo <= l and h <= hi)]
            b[1].append((lo, hi, op))
        for (name, lo, hi) in writes:
            b = self.bufs[name]
            b[0][:] = [(l, h, o) for (l, h, o) in b[0] if not (lo <= l and h <= hi)]
            b[1][:] = [(l, h, o) for (l, h, o) in b[1] if not (lo <= l and h <= hi)]
            b[0].append((lo, hi, op))
        if isdma:
            self.dma_count[dma] = self.dma_count.get(dma, 0) + 1
            op.dmaval = 16 * self.dma_count[dma]
        self.ops[eng].append(op)
        return op

    def emit(self, nc, final_waits=()):
        for e in ENGINES:
            c = 0
            for op in self.ops[e]:
                if op.dmakey is None and op.signal:
                    c += 1
                    op.val = c
        stats = {e: len(self.ops[e]) for e in ENGINES}
        with ExitStack() as st:
            esem = {e: st.enter_context(nc.semaphore("s_" + e)) for e in ENGINES}
            dsem = {k: st.enter_context(nc.semaphore("d_" + k)) for k in self.dma_count}
            block = st.enter_context(nc.Block())
            nwaits = {e: 0 for e in ENGINES}

            def make_body(e):
                def body(eng):
                    waited = {}
                    for op in self.ops[e]:
                        for pe_, o in op.deps.items():
                            key = ("e", pe_)
                            if waited.get(key, 0) < o.val:
                                eng.wait_ge(esem[pe_], o.val)
                                waited[key] = o.val
                                nwaits[e] += 1
                        for k, v in op.dmawaits.items():
                            key = ("d", k)
                            if waited.get(key, 0) < v:
                                eng.wait_ge(dsem[k], v)
                                waited[key] = v
                                nwaits[e] += 1
                        ins = op.fn(eng)
                        if op.dmakey is not None:
                            ins.then_inc(dsem[op.dmakey], 16)
                        elif op.signal:
                            ins.then_inc(esem[e], 1)
                    if e == SP:
                        for k in final_waits:
                            eng.wait_ge(dsem[k], 16 * self.dma_count[k])
                return body

            for e in ENGINES:
                getattr(block, e)(make_body(e))
        stats["waits"] = nwaits
        return stats


NSLOT = 3
DEBUG_STOP = None
CW = 2304
BW = 5120


def rYC(c, lo, hi):
    return ("A", c * NR1 + lo - R1LO, c * NR1 + hi - R1LO)


def rHT(b, j, lo, hi, t0):
    base = (b * 4 + j) * NR1
    return ("A", base + lo - t0, base + hi - t0)


def rCV(c, lo, hi):
    return ("A", c * OWN + lo - OWN0, c * OWN + hi - OWN0)


class Builder:
    def __init__(self, mode, unit_plan=None):
        assert mode in ("L0", "L1", "FUSED")
        self.mode = mode
        self.record = unit_plan is None
        self.plan = [] if unit_plan is None else unit_plan
        self.P = Prog(dry=self.record)
        self.uidx = 0
        self.issued = 0
        self.bank_i = 0
        self.rot = {}

    def bank(self):
        b = self.bank_i % 8
        self.bank_i += 1
        st = self.P.bufs.get("ps%d" % b)
        if st is not None and st[0] and not st[1]:
            raise RuntimeError("PSUM bank %d re-allocated before its last result was read (emission-order bug)" % b)
        return self.PS[b], ("ps%d" % b, 0, 1)

    def rotate(self, key, n):
        i = self.rot.get(key, 0)
        self.rot[key] = i + 1
        return i % n

    def Bv(self, lo, hi):
        return self.B[:, lo:hi], ("B", lo, hi)

    def Cv(self, lo, hi):
        return self.C[:, lo:hi], ("C", lo, hi)

    def _unit_src(self, desc):
        key, sub, off, n = desc[1]
        W = self.I[key] if sub is None else self.I[key][sub]
        if desc[0][0] == "k":
            return W.rearrange("(kc p) n -> p kc n", p=128)[:, :, off:off + n]
        return W.rearrange("(fc p) n -> p fc n", p=128)[:, off:off + n, :]

    def _slot_view(self, s, kind):
        slot = self.WR[s]
        if kind[0] == "k":
            return slot[:, 0:8 * kind[1]].rearrange("p (k n) -> p k n", k=8)
        return slot[:, 0:kind[1] * 1024].rearrange("p (f n) -> p f n", f=kind[1])

    def _issue_unit(self, i):
        kind = self.plan[i][0]
        src = self._unit_src(self.plan[i])
        s = i % NSLOT
        dst = self._slot_view(s, kind)
        self.P.add(POOL, lambda e, dst=dst, src=src: e.dma_start(out=dst, in_=src),
                   writes=[("W%d" % s, 0, 1)], dma="w%d" % s)

    def next_unit(self, kind, desc, live_prev=0):
        i = self.uidx
        self.uidx += 1
        if self.record:
            self.plan.append((kind, desc))
        else:
            assert self.plan[i] == (kind, desc), (i, self.plan[i], kind, desc)
            while self.issued < min(len(self.plan), i + NSLOT - live_prev):
                self._issue_unit(self.issued)
                self.issued += 1
        s = i % NSLOT
        return self._slot_view(s, kind), ("W%d" % s, 0, 1)

    def kunits(self, key, sub, ncols_total, c0=0, step=512):
        out = []
        c = c0
        while c < c0 + ncols_total:
            n = min(step, c0 + ncols_total - c)
            out.append((("k", n), (key, sub, c, n)))
            c += n
        return out

    def mm_group(self, ps, T, pairs, reads, pr):
        n = len(pairs)

        def mm(e):
            ins = None
            for i, (l, r) in enumerate(pairs):
                ins = e.matmul(ps[:, 0:T], lhsT=l, rhs=r, start=(i == 0), stop=(i == n - 1))
            return ins
        self.P.add(PE, mm, reads=reads, writes=[pr])

    def build(self):
        nc = bass.Bass("TRN2", target_bir_lowering=False)
        self.nc = nc
        mode = self.mode
        P = self.P
        dt = nc.dram_tensor
        I = {}
        if mode in ("L0", "FUSED"):
            I["xT"] = dt("xT", [D, N0], F32, kind="ExternalInput").ap()
        else:
            I["h1T"] = dt("h1T", [D, NR1], F32, kind="ExternalInput").ap()
        I["maskb"] = dt("maskb", [128, N0], F32, kind="ExternalInput").ap()
        I["memT"] = dt("memT", [D, MEM], F32, kind="ExternalInput").ap()
        I["vecs"] = dt("vecs", [128, NVEC, 8], F32, kind="ExternalInput").ap()
        I["ident"] = dt("ident", [128, 128], F32, kind="ExternalInput").ap()
        I["xa_wq"] = dt("xa_wq", [2, D, D], F32, kind="ExternalInput").ap()
        I["xa_wkv"] = dt("xa_wkv", [2, D, 2 * D], F32, kind="ExternalInput").ap()
        I["xa_wo"] = dt("xa_wo", [2, D, D], F32, kind="ExternalInput").ap()
        if mode in ("L0", "FUSED"):
            I["w_in"] = dt("w_in", [D, 2048], F32, kind="ExternalInput").ap()
            I["pool_w"] = dt("pool_w", [4, 128, 128], F32, kind="ExternalInput").ap()
            I["w_out"] = dt("w_out", [D, D], F32, kind="ExternalInput").ap()
            I["ffn_gu"] = dt("ffn_gu", [D, 2 * DFF], F32, kind="ExternalInput").ap()
            I["ffn_down"] = dt("ffn_down", [DFF, D], F32, kind="ExternalInput").ap()
        if mode in ("L1", "FUSED"):
            I["pw1"] = dt("pw1", [D, 2048], F32, kind="ExternalInput").ap()
            I["pw2"] = dt("pw2", [D, D], F32, kind="ExternalInput").ap()
            I["router"] = dt("router", [D, NEXP], F32, kind="ExternalInput").ap()
            I["moe_gu"] = dt("moe_gu", [NEXP, D, 2 * DFFE], F32, kind="ExternalInput").ap()
            I["moe_down"] = dt("moe_down", [NEXP, DFFE, D], F32, kind="ExternalInput").ap()
        if mode == "L0":
            OUT = dt("h1o", [D, NR1], F32, kind="ExternalOutput").ap()
        else:
            OUT = dt("outT", [D, OWN], F32, kind="ExternalOutput").ap()
        self.I = I

        with ExitStack() as st:
            sb = lambda name, shape, dtype: st.enter_context(nc.sbuf_tensor(name, shape, dtype))
            self.H = sb("H", [128, NCH, N0], F32)
            self.XN = sb("XN", [128, NCH, N0], BF16)
            self.MASK = sb("MASK", [128, N0], BF16)
            self.WR = [sb("WR%d" % i, [128, 4096], BF16) for i in range(NSLOT)]
            self.A = sb("A", [128, NCH * NR1], BF16)
            self.KT = sb("KT", [128, NCH, MEM], BF16)
            self.V = sb("V", [128, 2, D], BF16)
            self.VEC = sb("VEC", [128, NVEC, 8], F32)
            self.POOLW = sb("POOLW", [128, 4, 128], BF16)
            self.ONES = sb("ONES", [128, 128], BF16)
            self.ONESF = sb("ONESF", [128, 128], F32)
            self.IDENT = sb("IDENT", [128, 128], F32)
            self.EPSV = sb("EPSV", [128, 1], F32)
            self.RT = [sb("RT%d" % i, [128, 512], F32) for i in range(2)]
            self.RS = [sb("RS%d" % i, [128, 512], F32) for i in range(2)]
            self.B = sb("B", [128, BW], F32)
            self.C = sb("C", [128, CW], F32)
            self.PS = [st.enter_context(nc.psum_tensor("ps%d" % i, [128, 512], F32)) for i in range(8)]

            self.prologue()
            if mode in ("L0", "FUSED"):
                self.layer0()
            if mode in ("L1", "FUSED"):
                self.layer1()
            self.epilogue(OUT)
            if not self.record:
                self.stats = P.emit(nc, final_waits=["out"])
        return nc

    def prologue(self):
        P, I = self.P, self.I
        P.add(SP, lambda e: e.dma_start(out=self.VEC[:], in_=I["vecs"]), writes=[("VEC", 0, 1)], dma="c")
        P.add(SP, lambda e: e.dma_start(out=self.IDENT[:], in_=I["ident"]), writes=[("IDENT", 0, 1)], dma="c")
        hN = N0 // 2
        P.add(POOL, lambda e: e.dma_start(out=self.MASK[:, 0:hN], in_=I["maskb"][:, 0:hN]), writes=[("MASK", 0, hN)], dma="c2")
        P.add(POOL, lambda e: e.dma_start(out=self.MASK[:, hN:N0], in_=I["maskb"][:, hN:N0]), writes=[("MASK", hN, N0)], dma="c2")
        P.add(DVE, lambda e: e.memset(self.ONES[:], 1.0), writes=[("ONES", 0, 1)])
        P.add(DVE, lambda e: e.memset(self.ONESF[:], 1.0), writes=[("ONESF", 0, 1)])
        P.add(DVE, lambda e: e.memset(self.EPSV[:], EPS), writes=[("EPSV", 0, 1)])
        if self.mode in ("L0", "FUSED"):
            xv = I["xT"].rearrange("(c p) t -> p c t", p=128)
            for ti, (lo, hi) in enumerate(TN0):
                for c in range(NCH):
                    P.add(SP, lambda e, lo=lo, hi=hi, c=c: e.dma_start(out=self.H[:, c, lo:hi], in_=xv[:, c, lo:hi]),
                          writes=[("H%d" % c, lo, hi)], dma="x%d" % ti)
            P.add(POOL, lambda e: e.dma_start(out=self.POOLW[:], in_=I["pool_w"].rearrange("g c d -> c g d")),
                  writes=[("POOLW", 0, 1)], dma="c2")
        else:
            hv = I["h1T"].rearrange("(c p) t -> p c t", p=128)
            for c in range(NCH):
                P.add(SP, lambda e, c=c: e.dma_start(out=self.H[:, c, R1LO:R1HI], in_=hv[:, c, :]),
                      writes=[("H%d" % c, R1LO, R1HI)], dma="x")

    def epilogue(self, OUT):
        P = self.P
        ov = OUT.rearrange("(c p) t -> p c t", p=128)
        if self.mode == "L0":
            lo, hi = R1LO, R1HI
            for c in range(NCH):
                P.add(SP, lambda e, c=c: e.dma_start(out=ov[:, c, :], in_=self.H[:, c, lo:hi]),
                      reads=[("H%d" % c, lo, hi)], dma="out")
        else:
            for (lo, hi) in T1:
                for c in range(NCH):
                    P.add(SP, lambda e, lo=lo, hi=hi, c=c: e.dma_start(out=ov[:, c, lo - OWN0:hi - OWN0], in_=self.H[:, c, lo:hi]),
                          reads=[("H%d" % c, lo, hi)], dma="out")

    def rmsnorm(self, vidx, tiles, out_f32_inplace=False, save_rstd=False):
        P = self.P
        for (lo, hi) in tiles:
            T = hi - lo
            for c in range(NCH):
                P.add(ACT, lambda e, c=c, lo=lo, hi=hi: e.activation(out=self.XN[:, c, lo:hi], in_=self.H[:, c, lo:hi], func=AF.Square),
                      reads=[("H%d" % c, lo, hi)], writes=[("XN%d" % c, lo, hi)])
            ps, pr = self.bank()
            self.mm_group(ps, T, [(self.ONES[:, :], self.XN[:, c, lo:hi]) for c in range(NCH)],
                          [("XN%d" % c, lo, hi) for c in range(NCH)] + [("ONES", 0, 1)], pr)
            r = self.rotate("rt", 2)
            RT, RS = self.RT[r], self.RS[r]
            P.add(ACT, lambda e, ps=ps, RT=RT, T=T: e.activation(out=RT[:, 0:T], in_=ps[:, 0:T], func=AF.Ln, scale=1.0 / D, bias=self.EPSV[:, 0:1]),
                  reads=[pr, ("EPSV", 0, 1)], writes=[("RT%d" % r, 0, 1)])
            if save_rstd:
                rsap, rsr = self.Bv(lo - OWN0, hi - OWN0)
            else:
                rsap, rsr = RS[:, 0:T], ("RS%d" % r, 0, 1)
            P.add(ACT, lambda e, RT=RT, T=T, rsap=rsap: e.activation(out=rsap, in_=RT[:, 0:T], func=AF.Exp, scale=-0.5), reads=[("RT%d" % r, 0, 1)], writes=[rsr])
            for c in range(NCH):
                if out_f32_inplace:
                    out = self.H[:, c, lo:hi]
                    wr = [("H%d" % c, lo, hi)]
                else:
                    out = self.XN[:, c, lo:hi]
                    wr = [("XN%d" % c, lo, hi)]
                P.add(DVE, lambda e, c=c, lo=lo, hi=hi, out=out, rsap=rsap: e.scalar_tensor_tensor(
                    out=out, in0=self.H[:, c, lo:hi], scalar=self.VEC[:, vidx, c:c + 1], in1=rsap, op0=ALU.mult, op1=ALU.mult),
                    reads=[("H%d" % c, lo, hi), rsr, ("VEC", 0, 1)], writes=wr)

    def linear_residual(self, units, src, srcr, tiles, bias_vidx=None, src_off=0):
        P = self.P
        oc_base = 0
        for (kind, desc) in units:
            wv, wr = self.next_unit(kind, desc)
            nco = kind[1] // 128
            for j in range(nco):
                oc = oc_base + j
                for (lo, hi) in tiles:
                    T = hi - lo
                    ps, pr = self.bank()
                    self.mm_group(ps, T, [(wv[:, k, j * 128:(j + 1) * 128], src[:, k, lo - src_off:hi - src_off]) for k in range(NCH)],
                                  [wr] + [srcr(k, lo, hi) for k in range(NCH)], pr)
                    if bias_vidx is None:
                        P.add(DVE, lambda e, ps=ps, oc=oc, lo=lo, hi=hi, T=T: e.tensor_tensor(
                            out=self.H[:, oc, lo:hi], in0=ps[:, 0:T], in1=self.H[:, oc, lo:hi], op=ALU.add),
                            reads=[pr, ("H%d" % oc, lo, hi)], writes=[("H%d" % oc, lo, hi)])
                    else:
                        P.add(DVE, lambda e, ps=ps, oc=oc, lo=lo, hi=hi, T=T: e.scalar_tensor_tensor(
                            out=self.H[:, oc, lo:hi], in0=ps[:, 0:T], scalar=self.VEC[:, bias_vidx, oc:oc + 1], in1=self.H[:, oc, lo:hi],
                            op0=ALU.add, op1=ALU.add),
                            reads=[pr, ("H%d" % oc, lo, hi), ("VEC", 0, 1)], writes=[("H%d" % oc, lo, hi)])
            oc_base += nco

    def mixer0(self):
        P = self.P
        YC = self.A[:, :].rearrange("p (c t) -> p c t", c=NCH)
        W = 432
        T1s = [self.Bv(i * W, (i + 1) * W) for i in range(2)]
        GBs = [self.Bv((2 + i) * W, (3 + i) * W) for i in range(2)]
        Us = [self.Bv((4 + i) * W, (5 + i) * W) for i in range(2)]
        (Cb, rC), (T2b, rT2), (S1b, rS1), (S2b, rS2) = [self.Bv((6 + i) * W, (7 + i) * W) for i in range(4)]
        (K1b, rK1), (K2b, rK2), (PEb, rPE) = [self.Bv(10 * W + 64 * i, 10 * W + 64 * (i + 1)) for i in range(3)]
        PBs = []
        for i in range(2):
            f_, r_ = self.Cv(256 * i, 256 * (i + 1))
            PBs.append((f_.bitcast(BF16), r_))
        units = self.kunits("w_in", None, 2048)
        VEC = self.VEC
        pending = []
        for i in range(4):
            wv, wr = self.next_unit(*units[i])
            win = 2 << i
            left = win // 2
            for (lo, hi) in T0:
                T = hi - lo
                elo, ehi = lo - 8, hi + 7
                TE = ehi - elo
                banks = [self.bank() for _ in range(4)]
                for j in range(4):
                    ps, pr = banks[j]
                    self.mm_group(ps, TE, [(wv[:, k, j * 128:(j + 1) * 128], self.XN[:, k, elo:ehi]) for k in range(NCH)],
                                  [wr] + [("XN%d" % k, elo, ehi) for k in range(NCH)], pr)
                (ph, rh), (pgb, rgb), (pgc, rgc), (pu, ru) = banks
                while len(pending) > 0:
                    pending.pop(0)()
                bi_ = self.rotate("mx", 2)
                (T1b, rT1), (GBb, rGB), (Ub, rU) = T1s[bi_], GBs[bi_], Us[bi_]
                PBb, rPB = PBs[bi_]
                P.add(ACT, lambda e, ph=ph, TE=TE, T1b=T1b: e.copy(out=T1b[:, 0:TE], in_=ph[:, 0:TE]), reads=[rh], writes=[rT1])
                P.add(DVE, lambda e, pgc=pgc, TE=TE, T1b=T1b: e.tensor_tensor(out=Cb[:, 0:TE], in0=pgc[:, 0:TE], in1=T1b[:, 0:TE], op=ALU.mult),
                      reads=[rgc, rT1], writes=[rC])
                P.add(ACT, lambda e, pgb=pgb, T=T, GBb=GBb: e.copy(out=GBb[:, 0:T], in_=pgb[:, 8:8 + T]), reads=[rgb], writes=[rGB])
                P.add(ACT, lambda e, pu=pu, TE=TE, Ub=Ub: e.copy(out=Ub[:, 0:TE], in_=pu[:, 0:TE]), reads=[ru], writes=[rU])
                P.add(DVE, lambda e, T=T, i=i: e.tensor_scalar(out=T2b[:, 0:T], in0=Cb[:, 7:7 + T], scalar1=VEC[:, V_CONVA + 0, i:i + 1], scalar2=None, op0=ALU.mult),
                      reads=[rC, ("VEC", 0, 1)], writes=[rT2])
                for kk in (1, 2):
                    P.add(DVE, lambda e, T=T, i=i, kk=kk: e.scalar_tensor_tensor(
                        out=T2b[:, 0:T], in0=Cb[:, 7 + kk:7 + kk + T], scalar=VEC[:, V_CONVA + kk, i:i + 1], in1=T2b[:, 0:T],
                        op0=ALU.mult, op1=ALU.add), reads=[rC, rT2, ("VEC", 0, 1)], writes=[rT2])
                P.add(DVE, lambda e, T=T, i=i, lo=lo, hi=hi, GBb=GBb: e.tensor_tensor(out=YC[:, i, lo - R1LO:hi - R1LO], in0=T2b[:, 0:T], in1=GBb[:, 0:T], op=ALU.mult),
                      reads=[rT2, rGB], writes=[rYC(i, lo, hi)])
                srcU, rsU = Ub, rU
                n = TE
                step = 1
                bufs = [(S1b, rS1), (S2b, rS2)]
                bi = 0
                while step < win:
                    n2 = n - step
                    dS, rdS = bufs[bi]
                    P.add(DVE, lambda e, srcU=srcU, dS=dS, n2=n2, step=step: e.tensor_tensor(out=dS[:, 0:n2], in0=srcU[:, 0:n2], in1=srcU[:, step:step + n2], op=ALU.add),
                          reads=[rsU], writes=[rdS])
                    srcU, rsU = dS, rdS
                    n = n2
                    step *= 2
                    bi ^= 1
                off = 8 - left
                P.add(DVE, lambda e, srcU=srcU, off=off, T=T, win=win, Ub=Ub, PBb=PBb: e.scalar_tensor_tensor(out=PBb[:, 0:T], in0=srcU[:, off:off + T], scalar=1.0 / win, in1=Ub[:, 8:8 + T],
                                                                                             op0=ALU.mult, op1=ALU.subtract),
                      reads=[rsU, rU], writes=[rPB])
                for (a, b) in ((R1LO, R1LO + 32), (R1HI - 32, R1HI)):
                    if not (lo <= a and b <= hi):
                        continue
                    ma, mb = a - 8, b + 7
                    srcK, rsK = None, None
                    nk = mb - ma
                    stepk = 1
                    kb = [(K1b, rK1), (K2b, rK2)]
                    ki = 0
                    while stepk < win:
                        n2 = nk - stepk
                        dK, rdK = kb[ki]
                        if srcK is None:
                            P.add(DVE, lambda e, dK=dK, n2=n2, stepk=stepk, ma=ma: e.tensor_tensor(out=dK[:, 0:n2], in0=self.MASK[:, ma:ma + n2], in1=self.MASK[:, ma + stepk:ma + stepk + n2], op=ALU.add),
                                  reads=[("MASK", ma, mb)], writes=[rdK])
                        else:
                            P.add(DVE, lambda e, srcK=srcK, dK=dK, n2=n2, stepk=stepk: e.tensor_tensor(out=dK[:, 0:n2], in0=srcK[:, 0:n2], in1=srcK[:, stepk:stepk + n2], op=ALU.add),
                                  reads=[rsK], writes=[rdK])
                        srcK, rsK = dK, rdK
                        nk = n2
                        stepk *= 2
                        ki ^= 1
                    P.add(DVE, lambda e, srcK=srcK, off=off: e.tensor_scalar(out=PEb[:, 0:32], in0=srcK[:, off:off + 32], scalar1=1.0, scalar2=None, op0=ALU.max),
                          reads=[rsK], writes=[rPE])
                    P.add(DVE, lambda e: e.reciprocal(out=PEb[:, 0:32], in_=PEb[:, 0:32]), reads=[rPE], writes=[rPE])
                    eo = a - elo
                    P.add(DVE, lambda e, srcU=srcU, eo=eo, left=left: e.tensor_tensor(out=PEb[:, 0:32], in0=srcU[:, eo - left:eo - left + 32], in1=PEb[:, 0:32], op=ALU.mult),
                          reads=[rsU, rPE], writes=[rPE])
                    P.add(DVE, lambda e, eo=eo, a=a, lo=lo, Ub=Ub, PBb=PBb: e.tensor_tensor(out=PBb[:, a - lo:a - lo + 32], in0=PEb[:, 0:32], in1=Ub[:, eo:eo + 32], op=ALU.subtract),
                          reads=[rPE, rU, rPB], writes=[rPB])
                def pool_tail(T=T, i=i, lo=lo, hi=hi, PBb=PBb, rPB=rPB):
                    ps, pr = self.bank()
                    P.add(PE, lambda e, ps=ps: e.matmul(ps[:, 0:T], lhsT=self.POOLW[:, i, :], rhs=PBb[:, 0:T], start=True, stop=True),
                          reads=[rPB, ("POOLW", 0, 1)], writes=[pr])
                    P.add(ACT, lambda e, ps=ps: e.activation(out=YC[:, 4 + i, lo - R1LO:hi - R1LO], in_=ps[:, 0:T], func=AF.Identity,
                                                             scale=VEC[:, V_POOL_SCALE, i:i + 1]),
                          reads=[pr, ("VEC", 0, 1)], writes=[rYC(4 + i, lo, hi)])
                pending.append(pool_tail)
        while pending:
            pending.pop(0)()
        self.linear_residual(self.kunits("w_out", None, D), YC, rYC, T0, src_off=R1LO)

    def xattn(self, L, tiles, vnorm, vnormmem):
        P, I = self.P, self.I
        QT = self.A[:, :].rearrange("p (c t) -> p c t", c=NCH)
        MEMFf, rMEMF = self.Bv(0, 2048)
        MEMF = MEMFf.rearrange("p (c m) -> p c m", c=NCH)
        MEMNf, _ = self.Bv(2048, 3072)
        MEMN = MEMNf.bitcast(BF16).rearrange("p (c m) -> p c m", c=NCH)
        rMEMN = lambda c0, c1: ("B", 2048 + c0 * 128, 2048 + c1 * 128)
        ESf, _ = self.Bv(3072, 4096)
        ESb = ESf.bitcast(BF16)
        rES = lambda i: ("B", 3072 + i * 256, 3072 + (i + 1) * 256)
        P.add(SP, lambda e: e.dma_start(out=MEMF, in_=I["memT"].rearrange("(c p) m -> p c m", p=128)), writes=[rMEMF], dma="m%d" % L)
        for c in range(NCH):
            P.add(ACT, lambda e, c=c: e.activation(out=MEMN[:, c, :], in_=MEMF[:, c, :], func=AF.Square), reads=[rMEMF], writes=[rMEMN(c, c + 1)])
        ps, pr = self.bank()
        self.mm_group(ps, MEM, [(self.ONES[:, :], MEMN[:, c, :]) for c in range(NCH)], [rMEMN(0, NCH), ("ONES", 0, 1)], pr)
        r = self.rotate("rt", 2)
        RT, RS = self.RT[r], self.RS[r]
        P.add(ACT, lambda e, ps=ps, RT=RT: e.activation(out=RT[:, 0:MEM], in_=ps[:, 0:MEM], func=AF.Ln, scale=1.0 / D, bias=self.EPSV[:, 0:1]),
              reads=[pr, ("EPSV", 0, 1)], writes=[("RT%d" % r, 0, 1)])
        P.add(ACT, lambda e, RT=RT, RS=RS: e.activation(out=RS[:, 0:MEM], in_=RT[:, 0:MEM], func=AF.Exp, scale=-0.5), reads=[("RT%d" % r, 0, 1)], writes=[("RS%d" % r, 0, 1)])
        for c in range(NCH):
            P.add(DVE, lambda e, c=c, RS=RS: e.scalar_tensor_tensor(out=MEMN[:, c, :], in0=MEMF[:, c, :], scalar=self.VEC[:, vnormmem, c:c + 1], in1=RS[:, 0:MEM],
                                                                    op0=ALU.mult, op1=ALU.mult),
                  reads=[rMEMF, ("RS%d" % r, 0, 1), ("VEC", 0, 1)], writes=[rMEMN(c, c + 1)])
        kvunits = self.kunits("xa_wkv", L, 2 * D)
        for u in range(2):
            wv, wr = self.next_unit(*kvunits[u])
            for j in range(4):
                oc = u * 4 + j
                ps, pr = self.bank()
                self.mm_group(ps, MEM, [(wv[:, k, j * 128:(j + 1) * 128], MEMN[:, k, :]) for k in range(NCH)], [wr, rMEMN(0, NCH)], pr)
                P.add(ACT, lambda e, ps=ps, oc=oc: e.copy(out=self.KT[:, oc, :], in_=ps[:, 0:MEM]), reads=[pr], writes=[("KT", oc, oc + 1)])
        for u in range(2):
            wv, wr = self.next_unit(*kvunits[2 + u])
            for mc in range(2):
                ps, pr = self.bank()
                self.mm_group(ps, 512, [(MEMN[:, k, mc * 128:(mc + 1) * 128], wv[:, k, :]) for k in range(NCH)], [wr, rMEMN(0, NCH)], pr)
                P.add(DVE, lambda e, ps=ps, mc=mc, u=u: e.tensor_copy(out=self.V[:, mc, u * 512:(u + 1) * 512], in_=ps[:, 0:512]),
                      reads=[pr], writes=[("V", mc * 2 + u, mc * 2 + u + 1)])
        self.rmsnorm(vnorm, tiles)
        qunits = self.kunits("xa_wq", L, D)
        ev = 0
        for u in range(2):
            wv, wr = self.next_unit(*qunits[u])
            for j in range(4):
                oc = u * 4 + j
                for (lo, hi) in tiles:
                    T = hi - lo
                    ps, pr = self.bank()
                    self.mm_group(ps, T, [(wv[:, k, j * 128:(j + 1) * 128], self.XN[:, k, lo:hi]) for k in range(NCH)],
                                  [wr] + [("XN%d" % k, lo, hi) for k in range(NCH)], pr)
                    if ev % 2 == 0:
                        P.add(ACT, lambda e, ps=ps, oc=oc, lo=lo, hi=hi, T=T: e.copy(out=QT[:, oc, lo - R1LO:hi - R1LO], in_=ps[:, 0:T]),
                              reads=[pr], writes=[rYC(oc, lo, hi)])
                    else:
                        P.add(DVE, lambda e, ps=ps, oc=oc, lo=lo, hi=hi, T=T: e.tensor_copy(out=QT[:, oc, lo - R1LO:hi - R1LO], in_=ps[:, 0:T]),
                              reads=[pr], writes=[rYC(oc, lo, hi)])
                    ev += 1
        apend = []
        for (lo, hi) in tiles:
            T = hi - lo
            for h in range(4):
                es = self.rotate("es", 2)
                ES = [ESb[:, (es * 2 + mc) * 512:(es * 2 + mc) * 512 + 512] for mc in range(2)]
                rESl = [rES(es * 2 + mc) for mc in range(2)]
                RSM, rRSM = self.Bv(4096 + es * 512, 4096 + es * 512 + 512)
                for mc in range(2):
                    ps, pr = self.bank()
                    self.mm_group(ps, T, [(self.KT[:, 2 * h + hc, mc * 128:(mc + 1) * 128], QT[:, 2 * h + hc, lo - R1LO:hi - R1LO]) for hc in range(2)],
                                  [("KT", 2 * h, 2 * h + 2), rYC(2 * h, lo, hi), rYC(2 * h + 1, lo, hi)], pr)
                    P.add(ACT, lambda e, ps=ps, mc=mc, T=T, ES=ES: e.activation(out=ES[mc][:, 0:T], in_=ps[:, 0:T], func=AF.Exp, scale=1.0 / 16.0),
                          reads=[pr], writes=[rESl[mc]])
                while apend:
                    apend.pop(0)()

                def attn_tail(T=T, ES=ES, rESl=rESl, RSM=RSM, rRSM=rRSM, h=h, lo=lo, hi=hi):
                    ps, pr = self.bank()
                    self.mm_group(ps, T, [(self.ONES[:, :], ES[mc][:, 0:T]) for mc in range(2)], [rESl[0], rESl[1], ("ONES", 0, 1)], pr)
                    P.add(ACT, lambda e, ps=ps: e.activation(out=RSM[:, 0:T], in_=ps[:, 0:T], func=AF.Ln), reads=[pr], writes=[rRSM])
                    P.add(ACT, lambda e: e.activation(out=RSM[:, 0:T], in_=RSM[:, 0:T], func=AF.Exp, scale=-1.0), reads=[rRSM], writes=[rRSM])
                    for hc in range(2):
                        oc = 2 * h + hc
                        ps, pr = self.bank()
                        self.mm_group(ps, T, [(self.V[:, mc, oc * 128:(oc + 1) * 128], ES[mc][:, 0:T]) for mc in range(2)],
                                      [("V", 0, 4), rESl[0], rESl[1]], pr)
                        P.add(DVE, lambda e, ps=ps, oc=oc: e.tensor_tensor(out=QT[:, oc, lo - R1LO:hi - R1LO], in0=ps[:, 0:T], in1=RSM[:, 0:T], op=ALU.mult),
                              reads=[pr, rRSM], writes=[rYC(oc, lo, hi)])
                apend.append(attn_tail)
        while apend:
            apend.pop(0)()
        self.linear_residual(self.kunits("xa_wo", L, D), QT, rYC, tiles, src_off=R1LO)

    def swiglu(self, key, sub, dff, tiles, crow=None, crowr=None, mid_hook=None):
        P = self.P
        nfc = dff // 128
        HT = [self.A[:, b * 4 * NR1:(b + 1) * 4 * NR1].rearrange("p (j t) -> p j t", j=4) for b in range(2)]
        SGf, _ = self.Cv(0, 512)
        SGb = SGf.bitcast(BF16)
        rSG = lambda s: ("C", s * 256, (s + 1) * 256)
        t0 = tiles[0][0]
        g0 = 0
        while g0 < nfc:
            ng = min(4, nfc - g0)
            hb = self.rotate("ht", 2)
            wg, wgr = self.next_unit(("k", ng * 128), (key, sub, g0 * 128, ng * 128))
            wu, wur = self.next_unit(("k", ng * 128), (key, sub, dff + g0 * 128, ng * 128), live_prev=1)
            for j in range(ng):
                for (lo, hi) in tiles:
                    T = hi - lo
                    pg, pgr = self.bank()
                    pu, pur = self.bank()
                    for (ps, pr, wv, wr) in ((pg, pgr, wg, wgr), (pu, pur, wu, wur)):
                        self.mm_group(ps, T, [(wv[:, k, j * 128:(j + 1) * 128], self.XN[:, k, lo:hi]) for k in range(NCH)],
                                      [wr] + [("XN%d" % k, lo, hi) for k in range(NCH)], pr)
                    s = self.rotate("sg", 2)
                    SG = SGb[:, s * 512:s * 512 + 512]
                    P.add(ACT, lambda e, pg=pg, SG=SG, T=T: e.activation(out=SG[:, 0:T], in_=pg[:, 0:T], func=AF.Silu), reads=[pgr], writes=[rSG(s)])
                    hdst = HT[hb][:, j, lo - t0:hi - t0]
                    hr = rHT(hb, j, lo, hi, t0)
                    if crow is None:
                        P.add(DVE, lambda e, pu=pu, SG=SG, T=T, hdst=hdst: e.tensor_tensor(out=hdst, in0=pu[:, 0:T], in1=SG[:, 0:T], op=ALU.mult),
                              reads=[pur, rSG(s)], writes=[hr])
                    else:
                        TU, rTU = self.Cv(512 + s * 512, 1024 + s * 512)
                        P.add(DVE, lambda e, pu=pu, TU=TU, T=T, lo=lo, hi=hi: e.tensor_tensor(out=TU[:, 0:T], in0=pu[:, 0:T], in1=crow[:, lo - OWN0:hi - OWN0], op=ALU.mult),
                              reads=[pur, crowr(lo, hi)], writes=[rTU])
                        P.add(DVE, lambda e, SG=SG, TU=TU, T=T, hdst=hdst: e.tensor_tensor(out=hdst, in0=TU[:, 0:T], in1=SG[:, 0:T], op=ALU.mult),
                              reads=[rTU, rSG(s)], writes=[hr])
            if mid_hook is not None and g0 == 12:
                mid_hook()
            dkey = {"ffn_gu": "ffn_down", "moe_gu": "moe_down"}[key]
            wd, wdr = self.next_unit(("d", ng), (dkey, sub, g0, ng))
            for oc in range(NCH):
                for (lo, hi) in tiles:
                    T = hi - lo
                    ps, pr = self.bank()
                    self.mm_group(ps, T, [(wd[:, j, oc * 128:(oc + 1) * 128], HT[hb][:, j, lo - t0:hi - t0]) for j in range(ng)],
                                  [wdr] + [rHT(hb, j, lo, hi, t0) for j in range(ng)], pr)
                    P.add(DVE, lambda e, ps=ps, oc=oc, lo=lo, hi=hi, T=T: e.tensor_tensor(out=self.H[:, oc, lo:hi], in0=ps[:, 0:T], in1=self.H[:, oc, lo:hi], op=ALU.add),
                          reads=[pr, ("H%d" % oc, lo, hi)], writes=[("H%d" % oc, lo, hi)])
            g0 += ng

    def conformer(self):
        P = self.P
        VEC = self.VEC
        CV = self.A[:, 0:NCH * OWN].rearrange("p (c t) -> p c t", c=NCH)
        W = 448
        SIGs = [self.Cv(W * i, W * (i + 1)) for i in range(2)]
        HGfs = [self.Cv(W * (2 + i), W * (3 + i)) for i in range(2)]
        HGbf, _ = self.Cv(W * 4, W * 5)
        HGbb = HGbf.bitcast(BF16)
        rHGb = lambda i: ("C", W * 4 + i * 224, W * 4 + (i + 1) * 224)
        DGW = 31 * 64
        DGs = []
        for i in range(2):
            f, r = self.Bv(i * DGW, (i + 1) * DGW)
            DGs.append((f.bitcast(BF16).rearrange("p (k n) -> p k n", k=31), r))
        TN = [self.Bv(1024 + 512 * i, 1536 + 512 * i) for i in range(2)]
        self.rmsnorm(V_OD_NORM_MIX, T0)
        units = self.kunits("pw1", None, 2048)
        pending = []
        for u in range(4):
            wv, wr = self.next_unit(*units[u])
            for q in range(2):
                c = 2 * u + q
                dgi = self.rotate("dg31", 2)
                DG, rDG = DGs[dgi]
                for kk in range(31):
                    P.add(DVE, lambda e, kk=kk, c=c, DG=DG: e.tensor_scalar(out=DG[:, kk, :], in0=self.IDENT[:, :], scalar1=VEC[:, V_DW + kk, c:c + 1], scalar2=None, op0=ALU.mult),
                          reads=[("IDENT", 0, 1), ("VEC", 0, 1)], writes=[(rDG[0], rDG[1] + kk * 64, rDG[1] + (kk + 1) * 64)])
                for (lo, hi) in TC:
                    T = hi - lo
                    elo, ehi = lo - 15, hi + 15
                    TE = ehi - elo
                    pa, par = self.bank()
                    pg, pgr = self.bank()
                    for (ps, pr, col) in ((pa, par, q), (pg, pgr, 2 + q)):
                        self.mm_group(ps, TE, [(wv[:, k, col * 128:(col + 1) * 128], self.XN[:, k, elo:ehi]) for k in range(NCH)],
                                      [wr] + [("XN%d" % k, elo, ehi) for k in range(NCH)], pr)
                    while pending:
                        pending.pop(0)()
                    si = self.rotate("hg", 2)
                    (SIG, rSIG), (HGf, rHGf) = SIGs[si], HGfs[si]
                    HGb = HGbb[:, si * 448:(si + 1) * 448]
                    P.add(ACT, lambda e, pg=pg, c=c, TE=TE, SIG=SIG: e.activation(out=SIG[:, 0:TE], in_=pg[:, 0:TE], func=AF.Sigmoid, bias=VEC[:, V_PW1_BG, c:c + 1]),
                          reads=[pgr, ("VEC", 0, 1)], writes=[rSIG])
                    P.add(DVE, lambda e, pa=pa, c=c, TE=TE, HGf=HGf, SIG=SIG: e.scalar_tensor_tensor(out=HGf[:, 0:TE], in0=pa[:, 0:TE], scalar=VEC[:, V_PW1_BA, c:c + 1], in1=SIG[:, 0:TE],
                                                                                            op0=ALU.add, op1=ALU.mult),
                          reads=[par, rSIG, ("VEC", 0, 1)], writes=[rHGf])
                    P.add(DVE, lambda e, TE=TE, HGf=HGf, HGb=HGb, elo=elo, ehi=ehi: e.tensor_tensor(out=HGb[:, 0:TE], in0=HGf[:, 0:TE], in1=self.MASK[:, elo:ehi], op=ALU.mult),
                          reads=[rHGf, ("MASK", elo, ehi)], writes=[rHGb(si)])
                    def conv_tail(T=T, DG=DG, rDG=rDG, HGb=HGb, si=si, c=c, lo=lo, hi=hi):
                        ps, pr = self.bank()
                        self.mm_group(ps, T, [(DG[:, kk, :], HGb[:, kk:kk + T]) for kk in range(31)], [rDG, rHGb(si)], pr)
                        P.add(ACT, lambda e, ps=ps: e.activation(out=CV[:, c, lo - OWN0:hi - OWN0], in_=ps[:, 0:T], func=AF.Identity, bias=VEC[:, V_DW_B, c:c + 1]),
                              reads=[pr, ("VEC", 0, 1)], writes=[rCV(c, lo, hi)])
                    pending.append(conv_tail)
        while pending:
            pending.pop(0)()
        pw2u = self.kunits("pw2", None, D)
        pw2w = [self.next_unit(*pw2u[0]), self.next_unit(*pw2u[1], live_prev=1)]
        MUs = [self.Bv(0, 512), self.Bv(2048, 2560)]
        VARs = [self.Bv(512, 1024), self.Bv(2560, 3072)]
        st = {}

        def stage_a1(ti):
            lo, hi = TC[ti]
            T = hi - lo
            for c in range(NCH):
                P.add(ACT, lambda e, c=c: e.activation(out=self.XN[:, c, lo:hi], in_=CV[:, c, lo - OWN0:hi - OWN0], func=AF.Square),
                      reads=[rCV(c, lo, hi)], writes=[("XN%d" % c, lo, hi)])
            p1, p1r = self.bank()
            p2, p2r = self.bank()
            self.mm_group(p1, T, [(self.ONES[:, :], CV[:, c, lo - OWN0:hi - OWN0]) for c in range(NCH)], [rCV(c, lo, hi) for c in range(NCH)] + [("ONES", 0, 1)], p1r)
            self.mm_group(p2, T, [(self.ONES[:, :], self.XN[:, c, lo:hi]) for c in range(NCH)], [("XN%d" % c, lo, hi) for c in range(NCH)] + [("ONES", 0, 1)], p2r)
            r = self.rotate("rt", 2)
            st[ti] = (p1, p1r, p2, p2r, r)

        def stage_a2(ti):
            lo, hi = TC[ti]
            T = hi - lo
            p1, p1r, p2, p2r, r = st[ti]
            (MU, rMU), (VAR, rVAR) = MUs[ti % 2], VARs[ti % 2]
            RT, RS = self.RT[r], self.RS[r]
            P.add(ACT, lambda e: e.activation(out=MU[:, 0:T], in_=p1[:, 0:T], func=AF.Identity, scale=1.0 / D), reads=[p1r], writes=[rMU])
            P.add(DVE, lambda e: e.tensor_tensor(out=VAR[:, 0:T], in0=MU[:, 0:T], in1=MU[:, 0:T], op=ALU.mult), reads=[rMU], writes=[rVAR])
            P.add(DVE, lambda e: e.scalar_tensor_tensor(out=VAR[:, 0:T], in0=p2[:, 0:T], scalar=1.0 / D, in1=VAR[:, 0:T], op0=ALU.mult, op1=ALU.subtract),
                  reads=[p2r, rVAR], writes=[rVAR])
            P.add(DVE, lambda e: e.tensor_scalar(out=VAR[:, 0:T], in0=VAR[:, 0:T], scalar1=0.0, scalar2=None, op0=ALU.max),
                  reads=[rVAR], writes=[rVAR])
            P.add(ACT, lambda e: e.activation(out=RT[:, 0:T], in_=VAR[:, 0:T], func=AF.Ln, bias=self.EPSV[:, 0:1]),
                  reads=[rVAR, ("EPSV", 0, 1)], writes=[("RT%d" % r, 0, 1)])
            P.add(ACT, lambda e: e.activation(out=RS[:, 0:T], in_=RT[:, 0:T], func=AF.Exp, scale=-0.5), reads=[("RT%d" % r, 0, 1)], writes=[("RS%d" % r, 0, 1)])

        def stage_b(ti):
            lo, hi = TC[ti]
            T = hi - lo
            r = st[ti][4]
            (MU, rMU) = MUs[ti % 2]
            RS = self.RS[r]
            for c in range(NCH):
                tn = self.rotate("tn", 2)
                TNb, rTN = TN[tn]
                P.add(DVE, lambda e, c=c, TNb=TNb: e.tensor_tensor(out=TNb[:, 0:T], in0=CV[:, c, lo - OWN0:hi - OWN0], in1=MU[:, 0:T], op=ALU.subtract),
                      reads=[rCV(c, lo, hi), rMU], writes=[rTN])
                P.add(DVE, lambda e, TNb=TNb: e.tensor_tensor(out=TNb[:, 0:T], in0=TNb[:, 0:T], in1=RS[:, 0:T], op=ALU.mult),
                      reads=[rTN, ("RS%d" % r, 0, 1)], writes=[rTN])
                P.add(ACT, lambda e, c=c, TNb=TNb: e.activation(out=self.XN[:, c, lo:hi], in_=TNb[:, 0:T], func=AF.Silu,
                                                                scale=VEC[:, V_LN_G, c:c + 1], bias=VEC[:, V_LN_B, c:c + 1]),
                      reads=[rTN, ("VEC", 0, 1)], writes=[("XN%d" % c, lo, hi)])

        def stage_c(ti):
            lo, hi = TC[ti]
            T = hi - lo
            for u2 in range(2):
                wv2, wr2 = pw2w[u2]
                for j in range(4):
                    oc = u2 * 4 + j
                    ps, pr = self.bank()
                    self.mm_group(ps, T, [(wv2[:, k, j * 128:(j + 1) * 128], self.XN[:, k, lo:hi]) for k in range(NCH)],
                                  [wr2] + [("XN%d" % k, lo, hi) for k in range(NCH)], pr)
                    P.add(DVE, lambda e, ps=ps, oc=oc: e.scalar_tensor_tensor(
                        out=self.H[:, oc, lo:hi], in0=ps[:, 0:T], scalar=VEC[:, V_PW2_B, oc:oc + 1], in1=self.H[:, oc, lo:hi],
                        op0=ALU.add, op1=ALU.add),
                        reads=[pr, ("H%d" % oc, lo, hi), ("VEC", 0, 1)], writes=[("H%d" % oc, lo, hi)])

        nt = len(TC)
        stage_a1(0)
        stage_a2(0)
        for ti in range(nt):
            if ti + 1 < nt:
                stage_a1(ti + 1)
            stage_b(ti)
            if ti + 1 < nt:
                stage_a2(ti + 1)
            stage_c(ti)

    def moe(self):
        P, I = self.P, self.I
        VEC = self.VEC
        RSTDROW, _ = self.Bv(0, OWN)
        CROW, _ = self.Bv(OWN, 2 * OWN)
        rRSTD = lambda lo, hi: ("B", lo - OWN0, hi - OWN0)
        rCROW = lambda lo, hi: ("B", OWN + lo - OWN0, OWN + hi - OWN0)
        GRf, rGR = self.Cv(1536, 1600)
        GR = GRf.rearrange("p (c e) -> p c e", c=NCH)
        RTRf, rRTR = self.Cv(1600, 1664)
        RTR = RTRf.rearrange("p (c e) -> p c e", c=NCH)
        CBf, _ = self.Cv(1664, 1792)
        CBALL = CBf.rearrange("p (t e) -> p t e", t=16)
        rCB = lambda tt: ("C", 1664 + tt * 8, 1672 + tt * 8)
        (Lb, rL), (EQ1, rEQ1), (L2, rL2), (EQ2, rEQ2) = [self.Cv(1792 + 8 * i, 1800 + 8 * i) for i in range(4)]
        (M1, rM1), (M2, rM2), (DM, rDM), (EE, rEE), (P1, rP1), (P2, rP2), (RTOK, rRTOK) = [self.Cv(1824 + i, 1825 + i) for i in range(7)]
        DIAG = [self.Cv(1840 + 128 * i, 1968 + 128 * i) for i in range(2)]
        self.rmsnorm(V_NORM_MOE, T1, save_rstd=True)
        P.add(SP, lambda e: e.dma_start(out=RTR, in_=I["router"].rearrange("(c p) e -> p c e", p=128)), writes=[rRTR], dma="rt")
        for c in range(NCH):
            P.add(DVE, lambda e, c=c: e.tensor_scalar(out=GR[:, c, :], in0=RTR[:, c, :], scalar1=VEC[:, V_NORM_MOE, c:c + 1], scalar2=None, op0=ALU.mult),
                  reads=[rRTR, ("VEC", 0, 1)], writes=[("C", 1536 + 8 * c, 1544 + 8 * c)])
        for tt in range(16):
            lo = OWN0 + 128 * tt
            hi = lo + 128
            ps, pr = self.bank()
            self.mm_group(ps, NEXP, [(self.H[:, c, lo:hi], GR[:, c, :]) for c in range(NCH)], [("H%d" % c, lo, hi) for c in range(NCH)] + [rGR], pr)
            ps2, pr2 = self.bank()
            P.add(PE, lambda e, ps2=ps2, lo=lo, hi=hi: e.matmul(ps2[:, 0:1], lhsT=RSTDROW[0:1, lo - OWN0:hi - OWN0], rhs=self.ONESF[0:1, 0:1], start=True, stop=True),
                  reads=[rRSTD(lo, hi), ("ONESF", 0, 1)], writes=[pr2])
            P.add(ACT, lambda e, ps=ps: e.copy(out=Lb, in_=ps[:, 0:NEXP]), reads=[pr], writes=[rL])
            P.add(ACT, lambda e, ps2=ps2: e.copy(out=RTOK, in_=ps2[:, 0:1]), reads=[pr2], writes=[rRTOK])
            P.add(DVE, lambda e: e.reduce_max(out=M1, in_=Lb, axis=AX.X), reads=[rL], writes=[rM1])
            P.add(DVE, lambda e: e.tensor_scalar(out=EQ1, in0=Lb, scalar1=M1, scalar2=None, op0=ALU.is_equal), reads=[rL, rM1], writes=[rEQ1])
            P.add(DVE, lambda e: e.scalar_tensor_tensor(out=L2, in0=EQ1, scalar=-1.0e30, in1=Lb, op0=ALU.mult, op1=ALU.add), reads=[rEQ1, rL], writes=[rL2])
            P.add(DVE, lambda e: e.reduce_max(out=M2, in_=L2, axis=AX.X), reads=[rL2], writes=[rM2])
            P.add(DVE, lambda e: e.tensor_scalar(out=EQ2, in0=L2, scalar1=M2, scalar2=None, op0=ALU.is_equal), reads=[rL2, rM2], writes=[rEQ2])
            P.add(DVE, lambda e: e.tensor_tensor(out=DM, in0=M2, in1=M1, op=ALU.subtract), reads=[rM1, rM2], writes=[rDM])
            P.add(ACT, lambda e: e.activation(out=EE, in_=DM, func=AF.Exp, scale=RTOK), reads=[rDM, rRTOK], writes=[rEE])
            P.add(DVE, lambda e: e.tensor_scalar(out=P1, in0=EE, scalar1=1.0, scalar2=None, op0=ALU.add), reads=[rEE], writes=[rP1])
            P.add(DVE, lambda e: e.reciprocal(out=P1, in_=P1), reads=[rP1], writes=[rP1])
            P.add(DVE, lambda e: e.tensor_tensor(out=P2, in0=EE, in1=P1, op=ALU.mult), reads=[rEE, rP1], writes=[rP2])
            P.add(DVE, lambda e, tt=tt: e.tensor_scalar(out=CBALL[:, tt, :], in0=EQ1, scalar1=P1, scalar2=None, op0=ALU.mult),
                  reads=[rEQ1, rP1], writes=[rCB(tt)])
            P.add(DVE, lambda e, tt=tt: e.scalar_tensor_tensor(out=CBALL[:, tt, :], in0=EQ2, scalar=P2, in1=CBALL[:, tt, :], op0=ALU.mult, op1=ALU.add),
                  reads=[rEQ2, rP2, rCB(tt)], writes=[rCB(tt)])
        CROWS = [(CROW, rCROW), (RSTDROW, rRSTD)]

        def make_crow(ex):
            crow, crowr = CROWS[ex % 2]
            for tt in range(16):
                lo = OWN0 + 128 * tt
                hi = lo + 128
                dg = self.rotate("dg", 2)
                DG, rDG = DIAG[dg]
                P.add(DVE, lambda e, tt=tt, DG=DG: e.tensor_scalar(out=DG, in0=self.IDENT[:, :], scalar1=CBALL[:, tt, ex:ex + 1], scalar2=None, op0=ALU.mult),
                      reads=[("IDENT", 0, 1), rCB(tt)], writes=[rDG])
                ps, pr = self.bank()
                P.add(PE, lambda e, ps=ps, DG=DG: e.matmul(ps[:, 0:128], lhsT=self.ONESF[:, :], rhs=DG, start=True, stop=True),
                      reads=[rDG, ("ONESF", 0, 1)], writes=[pr])
                P.add(ACT, lambda e, ps=ps, lo=lo, hi=hi: e.copy(out=crow[:, lo - OWN0:hi - OWN0], in_=ps[:, 0:128]), reads=[pr], writes=[crowr(lo, hi)])

        make_crow(0)
        for ex in range(NEXP):
            crow, crowr = CROWS[ex % 2]
            hook = (lambda ex=ex: make_crow(ex + 1)) if ex + 1 < NEXP else None
            self.swiglu("moe_gu", ex, DFFE, T1, crow=crow, crowr=crowr, mid_hook=hook)

    def layer0(self):
        self.rmsnorm(V_EV_NORM_MIX, TN0)
        self.mixer0()
        if DEBUG_STOP == "mix":
            return
        self.xattn(0, T0, V_XA_NORM0, V_XA_NORMMEM0)
        if DEBUG_STOP == "xa0":
            return
        self.rmsnorm(V_EV_NORM_FFN, T0)
        self.swiglu("ffn_gu", None, DFF, T0)

    def layer1(self):
        self.conformer()
        if DEBUG_STOP == "conf":
            return
        self.xattn(1, T1, V_XA_NORM1, V_XA_NORMMEM1)
        if DEBUG_STOP == "xa1":
            return
        self.moe()
        self.rmsnorm(V_FINAL, T1, out_f32_inplace=True)


def build_program(mode):
    b1 = Builder(mode)
    b1.build()
    b2 = Builder(mode, unit_plan=b1.plan)
    nc = b2.build()
    return nc, b2


def _feat(v):
    return np.ascontiguousarray(np.asarray(v, np.float32).reshape(8, 128).T)


def _feat_small(v, n):
    out = np.zeros((128, 8), np.float32)
    out[:, :n] = np.asarray(v, np.float32).reshape(n, 128).T
    return out


def _prep_common(inp):
    vecs = np.zeros((128, NVEC, 8), np.float32)
    vecs[:, V_EV_NORM_MIX] = _feat(inp["ev_norm_mix"][0])
    vecs[:, V_EV_NORM_FFN] = _feat(inp["ev_norm_ffn"][0])
    vecs[:, V_XA_NORM0] = _feat(inp["xa_norm"][0])
    vecs[:, V_XA_NORMMEM0] = _feat(inp["xa_norm_mem"][0])
    vecs[:, V_XA_NORM1] = _feat(inp["xa_norm"][1])
    vecs[:, V_XA_NORMMEM1] = _feat(inp["xa_norm_mem"][1])
    vecs[:, V_OD_NORM_MIX] = _feat(inp["od_norm_mix"][0])
    vecs[:, V_PW1_BA] = _feat(inp["od_pw1_b"][0][:1024])
    vecs[:, V_PW1_BG] = _feat(inp["od_pw1_b"][0][1024:])
    vecs[:, V_DW_B] = _feat(inp["od_dw_b"][0])
    vecs[:, V_LN_G] = _feat(inp["od_ln_g"][0])
    vecs[:, V_LN_B] = _feat(inp["od_ln_b"][0])
    vecs[:, V_PW2_B] = _feat(inp["od_pw2_b"][0])
    vecs[:, V_NORM_MOE] = _feat(inp["od_norm_moe"][0])
    vecs[:, V_FINAL] = _feat(inp["final_norm"])
    vecs[:, V_POOL_SCALE] = _feat_small(inp["ev_pool_scale"][0], 4)
    for k in range(3):
        vecs[:, V_CONVA + k] = _feat_small(inp["ev_conv_a"][0][k], 4)
    for k in range(31):
        vecs[:, V_DW + k] = _feat(inp["od_dw_w"][0][k])
    perm_in = np.concatenate([np.concatenate([g * 512 + i * 128 + np.arange(128) for g in range(4)]) for i in range(4)])
    perm_pw1 = np.concatenate([np.concatenate([(2 * j) * 128 + np.arange(256), 1024 + (2 * j) * 128 + np.arange(256)]) for j in range(4)])
    f = lambda a: np.ascontiguousarray(np.asarray(a, np.float32))
    common = {
        "vecs": vecs,
        "ident": np.eye(128, dtype=np.float32),
        "xa_wq": f(inp["xa_wq"]), "xa_wkv": f(inp["xa_wkv"]), "xa_wo": f(inp["xa_wo"]),
        "w_in": f(np.asarray(inp["ev_w_in"][0])[:, perm_in]),
        "pool_w": f(inp["ev_pool_w"][0]),
        "w_out": f(inp["ev_w_out"][0]),
        "ffn_gu": f(inp["ev_ffn_gu"][0]),
        "ffn_down": f(inp["ev_ffn_down"][0]),
        "pw1": f(np.asarray(inp["od_pw1_w"][0])[:, perm_pw1]),
        "pw2": f(inp["od_pw2_w"][0]),
        "router": f(inp["od_router"][0]),
        "moe_gu": f(inp["od_moe_gu"][0]),
        "moe_down": f(inp["od_moe_down"][0]),
    }
    return common


def _prep_core(inp, core):
    b, half = core // 2, core % 2
    s0 = half * OWN
    x = np.asarray(inp["x"], np.float32)
    xT = np.zeros((D, N0), np.float32)
    mask = np.zeros((N0,), np.float32)
    g0, g1 = s0 - HALO, s0 + OWN + HALO
    v0, v1 = max(g0, 0), min(g1, SEQ)
    xT[:, v0 - g0:v1 - g0] = x[b, v0:v1, :].T
    mask[v0 - g0:v1 - g0] = 1.0
    memT = np.ascontiguousarray(np.asarray(inp["mem"], np.float32)[b].T)
    return {"xT": xT, "maskb": np.ascontiguousarray(np.broadcast_to(mask, (128, N0))), "memT": memT}


_L0_KEYS = ["xT", "maskb", "memT", "vecs", "ident", "xa_wq", "xa_wkv", "xa_wo", "w_in", "pool_w", "w_out", "ffn_gu", "ffn_down"]
_L1_KEYS = ["h1T", "maskb", "memT", "vecs", "ident", "xa_wq", "xa_wkv", "xa_wo", "pw1", "pw2", "router", "moe_gu", "moe_down"]
_FUSED_KEYS = ["xT", "maskb", "memT", "vecs", "ident", "xa_wq", "xa_wkv", "xa_wo", "w_in", "pool_w", "w_out", "ffn_gu", "ffn_down",
               "pw1", "pw2", "router", "moe_gu", "moe_down"]

_CACHE = {}
FUSED = True


def _get_prog(mode):
    if mode not in _CACHE:
        _CACHE[mode] = build_program(mode)[0]
    return _CACHE[mode]


def kernel(**inputs):
    common = _prep_common(inputs)
    cores = [dict(common, **_prep_core(inputs, i)) for i in range(8)]
    out = np.zeros((4, SEQ, D), np.float32)
    if FUSED:
        nc = _get_prog("FUSED")
        res = run_bass_kernel_spmd(nc, [{k: c[k] for k in _FUSED_KEYS} for c in cores], core_ids=list(range(8)))
        outs = [r["outT"] for r in res.results]
    else:
        nc0 = _get_prog("L0")
        res0 = run_bass_kernel_spmd(nc0, [{k: c[k] for k in _L0_KEYS} for c in cores], core_ids=list(range(8)))
        for i in range(8):
            cores[i]["h1T"] = np.asarray(res0.results[i]["h1o"])
        nc1 = _get_prog("L1")
        res1 = run_bass_kernel_spmd(nc1, [{k: c[k] for k in _L1_KEYS} for c in cores], core_ids=list(range(8)))
        outs = [r["outT"] for r in res1.results]
    for i in range(8):
        b, half = i // 2, i % 2
        out[b, half * OWN:(half + 1) * OWN, :] = np.asarray(outs[i]).T
    return out
```

```python
from contextlib import ExitStack
import numpy as np
import concourse.bass as bass
import concourse.mybir as mybir
from concourse.bass_utils import run_bass_kernel_spmd

F32 = mybir.dt.float32
BF16 = mybir.dt.bfloat16
AF = mybir.ActivationFunctionType
ALU = mybir.AluOpType
AX = mybir.AxisListType

PE, ACT, DVE, POOL, SP = "tensor", "scalar", "vector", "gpsimd", "sync"
ENGINES = (PE, ACT, DVE, POOL, SP)

D = 1024
NCH = 8
SEQ = 4096
OWN = 2048
HALO = 24
N0 = OWN + 2 * HALO
OWN0 = HALO
R1LO, R1HI = 8, 2088
NR1 = R1HI - R1LO
T0 = [(R1LO + 416 * i, R1LO + 416 * (i + 1)) for i in range(5)]
T1 = [(OWN0 + 512 * i, OWN0 + 512 * (i + 1)) for i in range(4)]
TC = [(OWN0 + 410 * i, min(OWN0 + 410 * (i + 1), OWN0 + OWN)) for i in range(5)]
TN0 = [(0, 420), (420, 840), (840, 1260), (1260, 1680), (1680, 2096)]
MEM = 256
DFF = 2816
DFFE = 3584
NEXP = 8
EPS = 1e-6

V_EV_NORM_MIX, V_EV_NORM_FFN, V_XA_NORM0, V_XA_NORMMEM0, V_XA_NORM1, V_XA_NORMMEM1 = 0, 1, 2, 3, 4, 5
V_OD_NORM_MIX, V_PW1_BA, V_PW1_BG, V_DW_B, V_LN_G, V_LN_B, V_PW2_B, V_NORM_MOE, V_FINAL = 6, 7, 8, 9, 10, 11, 12, 13, 14
V_POOL_SCALE = 15
V_CONVA = 16
V_DW = 19
NVEC = 50


class Op:
    __slots__ = ("eng", "fn", "deps", "dmakey", "dmaval", "signal", "val", "idx", "dmawaits")

    def __init__(self, eng, fn, dmakey, idx):
        self.eng = eng
        self.fn = fn
        self.deps = {}
        self.dmawaits = {}
        self.dmakey = dmakey
        self.dmaval = 0
        self.signal = False
        self.val = 0
        self.idx = idx


class Prog:
    def __init__(self, dry=False):
        self.ops = {e: [] for e in ENGINES}
        self.bufs = {}
        self.dma_count = {}
        self.n = 0
        self.dry = dry

    def add(self, eng, fn, reads=(), writes=(), dma=None):
        if self.dry:
            return None
        op = Op(eng, fn, dma, self.n)
        self.n += 1
        deps = []
        for (name, lo, hi) in reads:
            b = self.bufs.get(name)
            if b is None:
                b = self.bufs[name] = ([], [])
            for (l, h, o) in b[0]:
                if l < hi and lo < h:
                    deps.append(o)
        for (name, lo, hi) in writes:
            b = self.bufs.get(name)
            if b is None:
                b = self.bufs[name] = ([], [])
            for (l, h, o) in b[0]:
                if l < hi and lo < h:
                    deps.append(o)
            for (l, h, o) in b[1]:
                if l < hi and lo < h:
                    deps.append(o)
        for o in deps:
            if o.dmakey is not None:
                v = 16 * self.dma_count[o.dmakey]
                if op.dmawaits.get(o.dmakey, 0) < v:
                    op.dmawaits[o.dmakey] = v
            else:
                if o.eng == PE and eng == PE:
                    continue
                cur = op.deps.get(o.eng)
                if cur is None or cur.idx < o.idx:
                    op.deps[o.eng] = o
        for o in op.deps.values():
            o.signal = True
        isdma = dma is not None
        for (name, lo, hi) in reads:
            b = self.bufs[name]
            if not isdma:
                b[1][:] = [(l, h, o) for (l, h, o) in b[1]
                           if not (o.eng == eng and o.dmakey is None and lo <= l and h <= hi)]
            b[1].append((lo, hi, op))
        for (name, lo, hi) in writes:
            b = self.bufs[name]
            b[0][:] = [(l, h, o) for (l, h, o) in b[0] if not (lo <= l and h <= hi)]
            b[1][:] = [(l, h, o) for (l, h, o) in b[1] if not (lo <= l and h <= hi)]
            b[0].append((lo, hi, op))
        if isdma:
            self.dma_count[dma] = self.dma_count.get(dma, 0) + 1
            op.dmaval = 16 * self.dma_count[dma]
        self.ops[eng].append(op)
        return op

    def emit(self, nc, final_waits=()):
        for e in ENGINES:
            c = 0
            for op in self.ops[e]:
                if op.dmakey is None and op.signal:
                    c += 1
                    op.val = c
        stats = {e: len(self.ops[e]) for e in ENGINES}
        with ExitStack() as st:
            esem = {e: st.enter_context(nc.semaphore("s_" + e)) for e in ENGINES}
            dsem = {k: st.enter_context(nc.semaphore("d_" + k)) for k in self.dma_count}
            block = st.enter_context(nc.Block())
            nwaits = {e: 0 for e in ENGINES}

            def make_body(e):
                def body(eng):
                    waited = {}
                    for op in self.ops[e]:
                        for pe_, o in op.deps.items():
                            key = ("e", pe_)
                            if waited.get(key, 0) < o.val:
                                eng.wait_ge(esem[pe_], o.val)
                                waited[key] = o.val
                                nwaits[e] += 1
                        for k, v in op.dmawaits.items():
                            key = ("d", k)
                            if waited.get(key, 0) < v:
                                eng.wait_ge(dsem[k], v)
                                waited[key] = v
                                nwaits[e] += 1
                        ins = op.fn(eng)
                        if op.dmakey is not None:
                            ins.then_inc(dsem[op.dmakey], 16)
                        elif op.signal:
                            ins.then_inc(esem[e], 1)
                    if e == SP:
                        for k in final_waits:
                            eng.wait_ge(dsem[k], 16 * self.dma_count[k])
                return body

            for e in ENGINES:
                getattr(block, e)(make_body(e))
        stats["waits"] = nwaits
        return stats


NSLOT = 3
DEBUG_STOP = None
CW = 2304
BW = 5120


def rYC(c, lo, hi):
    return ("A", c * NR1 + lo - R1LO, c * NR1 + hi - R1LO)


def rHT(b, j, lo, hi, t0):
    base = (b * 4 + j) * NR1
    return ("A", base + lo - t0, base + hi - t0)


def rCV(c, lo, hi):
    return ("A", c * OWN + lo - OWN0, c * OWN + hi - OWN0)


class Builder:
    def __init__(self, mode, unit_plan=None):
        assert mode in ("L0", "L1", "FUSED")
        self.mode = mode
        self.record = unit_plan is None
        self.plan = [] if unit_plan is None else unit_plan
        self.P = Prog(dry=self.record)
        self.uidx = 0
        self.issued = 0
        self.bank_i = 0
        self.rot = {}

    def bank(self):
        b = self.bank_i % 8
        self.bank_i += 1
        st = self.P.bufs.get("ps%d" % b)
        if st is not None and st[0] and not st[1]:
            raise RuntimeError("PSUM bank %d re-allocated before its last result was read (emission-order bug)" % b)
        return self.PS[b], ("ps%d" % b, 0, 1)

    def rotate(self, key, n):
        i = self.rot.get(key, 0)
        self.rot[key] = i + 1
        return i % n

    def Bv(self, lo, hi):
        return self.B[:, lo:hi], ("B", lo, hi)

    def Cv(self, lo, hi):
        return self.C[:, lo:hi], ("C", lo, hi)

    def _unit_src(self, desc):
        key, sub, off, n = desc[1]
        W = self.I[key] if sub is None else self.I[key][sub]
        if desc[0][0] == "k":
            return W.rearrange("(kc p) n -> p kc n", p=128)[:, :, off:off + n]
        return W.rearrange("(fc p) n -> p fc n", p=128)[:, off:off + n, :]

    def _slot_view(self, s, kind):
        slot = self.WR[s]
        if kind[0] == "k":
            return slot[:, 0:8 * kind[1]].rearrange("p (k n) -> p k n", k=8)
        return slot[:, 0:kind[1] * 1024].rearrange("p (f n) -> p f n", f=kind[1])

    def _issue_unit(self, i):
        kind = self.plan[i][0]
        src = self._unit_src(self.plan[i])
        s = i % NSLOT
        dst = self._slot_view(s, kind)
        self.P.add(POOL, lambda e, dst=dst, src=src: e.dma_start(out=dst, in_=src),
                   writes=[("W%d" % s, 0, 1)], dma="w%d" % s)

    def next_unit(self, kind, desc, live_prev=0):
        i = self.uidx
        self.uidx += 1
        if self.record:
            self.plan.append((kind, desc))
        else:
            assert self.plan[i] == (kind, desc), (i, self.plan[i], kind, desc)
            while self.issued < min(len(self.plan), i + NSLOT - live_prev):
                self._issue_unit(self.issued)
                self.issued += 1
        s = i % NSLOT
        return self._slot_view(s, kind), ("W%d" % s, 0, 1)

    def kunits(self, key, sub, ncols_total, c0=0, step=512):
        out = []
        c = c0
        while c < c0 + ncols_total:
            n = min(step, c0 + ncols_total - c)
            out.append((("k", n), (key, sub, c, n)))
            c += n
        return out

    def mm_group(self, ps, T, pairs, reads, pr):
        n = len(pairs)

        def mm(e):
            ins = None
            for i, (l, r) in enumerate(pairs):
                ins = e.matmul(ps[:, 0:T], lhsT=l, rhs=r, start=(i == 0), stop=(i == n - 1))
            return ins
        self.P.add(PE, mm, reads=reads, writes=[pr])

    def build(self):
        nc = bass.Bass("TRN2", target_bir_lowering=False)
        self.nc = nc
        mode = self.mode
        P = self.P
        dt = nc.dram_tensor
        I = {}
        if mode in ("L0", "FUSED"):
            I["xT"] = dt("xT", [D, N0], F32, kind="ExternalInput").ap()
        else:
            I["h1T"] = dt("h1T", [D, NR1], F32, kind="ExternalInput").ap()
        I["maskb"] = dt("maskb", [128, N0], F32, kind="ExternalInput").ap()
        I["memT"] = dt("memT", [D, MEM], F32, kind="ExternalInput").ap()
        I["vecs"] = dt("vecs", [128, NVEC, 8], F32, kind="ExternalInput").ap()
        I["ident"] = dt("ident", [128, 128], F32, kind="ExternalInput").ap()
        I["xa_wq"] = dt("xa_wq", [2, D, D], F32, kind="ExternalInput").ap()
        I["xa_wkv"] = dt("xa_wkv", [2, D, 2 * D], F32, kind="ExternalInput").ap()
        I["xa_wo"] = dt("xa_wo", [2, D, D], F32, kind="ExternalInput").ap()
        if mode in ("L0", "FUSED"):
            I["w_in"] = dt("w_in", [D, 2048], F32, kind="ExternalInput").ap()
            I["pool_w"] = dt("pool_w", [4, 128, 128], F32, kind="ExternalInput").ap()
            I["w_out"] = dt("w_out", [D, D], F32, kind="ExternalInput").ap()
            I["ffn_gu"] = dt("ffn_gu", [D, 2 * DFF], F32, kind="ExternalInput").ap()
            I["ffn_down"] = dt("ffn_down", [DFF, D], F32, kind="ExternalInput").ap()
        if mode in ("L1", "FUSED"):
            I["pw1"] = dt("pw1", [D, 2048], F32, kind="ExternalInput").ap()
            I["pw2"] = dt("pw2", [D, D], F32, kind="ExternalInput").ap()
            I["router"] = dt("router", [D, NEXP], F32, kind="ExternalInput").ap()
            I["moe_gu"] = dt("moe_gu", [NEXP, D, 2 * DFFE], F32, kind="ExternalInput").ap()
            I["moe_down"] = dt("moe_down", [NEXP, DFFE, D], F32, kind="ExternalInput").ap()
        if mode == "L0":
            OUT = dt("h1o", [D, NR1], F32, kind="ExternalOutput").ap()
        else:
            OUT = dt("outT", [D, OWN], F32, kind="ExternalOutput").ap()
        self.I = I

        with ExitStack() as st:
            sb = lambda name, shape, dtype: st.enter_context(nc.sbuf_tensor(name, shape, dtype))
            self.H = sb("H", [128, NCH, N0], F32)
            self.XN = sb("XN", [128, NCH, N0], BF16)
            self.MASK = sb("MASK", [128, N0], BF16)
            self.WR = [sb("WR%d" % i, [128, 4096], BF16) for i in range(NSLOT)]
            self.A = sb("A", [128, NCH * NR1], BF16)
            self.KT = sb("KT", [128, NCH, MEM], BF16)
            self.V = sb("V", [128, 2, D], BF16)
            self.VEC = sb("VEC", [128, NVEC, 8], F32)
            self.POOLW = sb("POOLW", [128, 4, 128], BF16)
            self.ONES = sb("ONES", [128, 128], BF16)
            self.ONESF = sb("ONESF", [128, 128], F32)
            self.IDENT = sb("IDENT", [128, 128], F32)
            self.EPSV = sb("EPSV", [128, 1], F32)
            self.RT = [sb("RT%d" % i, [128, 512], F32) for i in range(2)]
            self.RS = [sb("RS%d" % i, [128, 512], F32) for i in range(2)]
            self.B = sb("B", [128, BW], F32)
            self.C = sb("C", [128, CW], F32)
            self.PS = [st.enter_context(nc.psum_tensor("ps%d" % i, [128, 512], F32)) for i in range(8)]

            self.prologue()
            if mode in ("L0", "FUSED"):
                self.layer0()
            if mode in ("L1", "FUSED"):
                self.layer1()
            self.epilogue(OUT)
            if not self.record:
                self.stats = P.emit(nc, final_waits=["out"])
        return nc

    def prologue(self):
        P, I = self.P, self.I
        P.add(SP, lambda e: e.dma_start(out=self.VEC[:], in_=I["vecs"]), writes=[("VEC", 0, 1)], dma="c")
        P.add(SP, lambda e: e.dma_start(out=self.IDENT[:], in_=I["ident"]), writes=[("IDENT", 0, 1)], dma="c")
        hN = N0 // 2
        P.add(POOL, lambda e: e.dma_start(out=self.MASK[:, 0:hN], in_=I["maskb"][:, 0:hN]), writes=[("MASK", 0, hN)], dma="c2")
        P.add(POOL, lambda e: e.dma_start(out=self.MASK[:, hN:N0], in_=I["maskb"][:, hN:N0]), writes=[("MASK", hN, N0)], dma="c2")
        P.add(DVE, lambda e: e.memset(self.ONES[:], 1.0), writes=[("ONES", 0, 1)])
        P.add(DVE, lambda e: e.memset(self.ONESF[:], 1.0), writes=[("ONESF", 0, 1)])
        P.add(DVE, lambda e: e.memset(self.EPSV[:], EPS), writes=[("EPSV", 0, 1)])
        if self.mode in ("L0", "FUSED"):
            xv = I["xT"].rearrange("(c p) t -> p c t", p=128)
            for ti, (lo, hi) in enumerate(TN0):
                for c in range(NCH):
                    P.add(SP, lambda e, lo=lo, hi=hi, c=c: e.dma_start(out=self.H[:, c, lo:hi], in_=xv[:, c, lo:hi]),
                          writes=[("H%d" % c, lo, hi)], dma="x%d" % ti)
            P.add(POOL, lambda e: e.dma_start(out=self.POOLW[:], in_=I["pool_w"].rearrange("g c d -> c g d")),
                  writes=[("POOLW", 0, 1)], dma="c2")
        else:
            hv = I["h1T"].rearrange("(c p) t -> p c t", p=128)
            for c in range(NCH):
                P.add(SP, lambda e, c=c: e.dma_start(out=self.H[:, c, R1LO:R1HI], in_=hv[:, c, :]),
                      writes=[("H%d" % c, R1LO, R1HI)], dma="x")

    def epilogue(self, OUT):
        P = self.P
        ov = OUT.rearrange("(c p) t -> p c t", p=128)
        if self.mode == "L0":
            lo, hi = R1LO, R1HI
            for c in range(NCH):
                P.add(SP, lambda e, c=c: e.dma_start(out=ov[:, c, :], in_=self.H[:, c, lo:hi]),
                      reads=[("H%d" % c, lo, hi)], dma="out")
        else:
            for (lo, hi) in T1:
                for c in range(NCH):
                    P.add(SP, lambda e, lo=lo, hi=hi, c=c: e.dma_start(out=ov[:, c, lo - OWN0:hi - OWN0], in_=self.H[:, c, lo:hi]),
                          reads=[("H%d" % c, lo, hi)], dma="out")

    def rmsnorm(self, vidx, tiles, out_f32_inplace=False, save_rstd=False):
        P = self.P
        for (lo, hi) in tiles:
            T = hi - lo
            for c in range(NCH):
                P.add(ACT, lambda e, c=c, lo=lo, hi=hi: e.activation(out=self.XN[:, c, lo:hi], in_=self.H[:, c, lo:hi], func=AF.Square),
                      reads=[("H%d" % c, lo, hi)], writes=[("XN%d" % c, lo, hi)])
            ps, pr = self.bank()
            self.mm_group(ps, T, [(self.ONES[:, :], self.XN[:, c, lo:hi]) for c in range(NCH)],
                          [("XN%d" % c, lo, hi) for c in range(NCH)] + [("ONES", 0, 1)], pr)
            r = self.rotate("rt", 2)
            RT, RS = self.RT[r], self.RS[r]
            P.add(ACT, lambda e, ps=ps, RT=RT, T=T: e.activation(out=RT[:, 0:T], in_=ps[:, 0:T], func=AF.Ln, scale=1.0 / D, bias=self.EPSV[:, 0:1]),
                  reads=[pr, ("EPSV", 0, 1)], writes=[("RT%d" % r, 0, 1)])
            if save_rstd:
                rsap, rsr = self.Bv(lo - OWN0, hi - OWN0)
            else:
                rsap, rsr = RS[:, 0:T], ("RS%d" % r, 0, 1)
            P.add(ACT, lambda e, RT=RT, T=T, rsap=rsap: e.activation(out=rsap, in_=RT[:, 0:T], func=AF.Exp, scale=-0.5), reads=[("RT%d" % r, 0, 1)], writes=[rsr])
            for c in range(NCH):
                if out_f32_inplace:
                    out = self.H[:, c, lo:hi]
                    wr = [("H%d" % c, lo, hi)]
                else:
                    out = self.XN[:, c, lo:hi]
                    wr = [("XN%d" % c, lo, hi)]
                P.add(DVE, lambda e, c=c, lo=lo, hi=hi, out=out, rsap=rsap: e.scalar_tensor_tensor(
                    out=out, in0=self.H[:, c, lo:hi], scalar=self.VEC[:, vidx, c:c + 1], in1=rsap, op0=ALU.mult, op1=ALU.mult),
                    reads=[("H%d" % c, lo, hi), rsr, ("VEC", 0, 1)], writes=wr)

    def linear_residual(self, units, src, srcr, tiles, bias_vidx=None, src_off=0):
        P = self.P
        oc_base = 0
        for (kind, desc) in units:
            wv, wr = self.next_unit(kind, desc)
            nco = kind[1] // 128
            for j in range(nco):
                oc = oc_base + j
                for (lo, hi) in tiles:
                    T = hi - lo
                    ps, pr = self.bank()
                    self.mm_group(ps, T, [(wv[:, k, j * 128:(j + 1) * 128], src[:, k, lo - src_off:hi - src_off]) for k in range(NCH)],
                                  [wr] + [srcr(k, lo, hi) for k in range(NCH)], pr)
                    if bias_vidx is None:
                        P.add(DVE, lambda e, ps=ps, oc=oc, lo=lo, hi=hi, T=T: e.tensor_tensor(
                            out=self.H[:, oc, lo:hi], in0=ps[:, 0:T], in1=self.H[:, oc, lo:hi], op=ALU.add),
                            reads=[pr, ("H%d" % oc, lo, hi)], writes=[("H%d" % oc, lo, hi)])
                    else:
                        P.add(DVE, lambda e, ps=ps, oc=oc, lo=lo, hi=hi, T=T: e.scalar_tensor_tensor(
                            out=self.H[:, oc, lo:hi], in0=ps[:, 0:T], scalar=self.VEC[:, bias_vidx, oc:oc + 1], in1=self.H[:, oc, lo:hi],
                            op0=ALU.add, op1=ALU.add),
                            reads=[pr, ("H%d" % oc, lo, hi), ("VEC", 0, 1)], writes=[("H%d" % oc, lo, hi)])
            oc_base += nco

    def mixer0(self):
        P = self.P
        YC = self.A[:, :].rearrange("p (c t) -> p c t", c=NCH)
        W = 432
        T1s = [self.Bv(i * W, (i + 1) * W) for i in range(2)]
        GBs = [self.Bv((2 + i) * W, (3 + i) * W) for i in range(2)]
        Us = [self.Bv((4 + i) * W, (5 + i) * W) for i in range(2)]
        (Cb, rC), (T2b, rT2), (S1b, rS1), (S2b, rS2) = [self.Bv((6 + i) * W, (7 + i) * W) for i in range(4)]
        (K1b, rK1), (K2b, rK2), (PEb, rPE) = [self.Bv(10 * W + 64 * i, 10 * W + 64 * (i + 1)) for i in range(3)]
        PBs = []
        for i in range(2):
            f_, r_ = self.Cv(256 * i, 256 * (i + 1))
            PBs.append((f_.bitcast(BF16), r_))
        units = self.kunits("w_in", None, 2048)
        VEC = self.VEC
        pending = []
        for i in range(4):
            wv, wr = self.next_unit(*units[i])
            win = 2 << i
            left = win // 2
            for (lo, hi) in T0:
                T = hi - lo
                elo, ehi = lo - 8, hi + 7
                TE = ehi - elo
                banks = [self.bank() for _ in range(4)]
                for j in range(4):
                    ps, pr = banks[j]
                    self.mm_group(ps, TE, [(wv[:, k, j * 128:(j + 1) * 128], self.XN[:, k, elo:ehi]) for k in range(NCH)],
                                  [wr] + [("XN%d" % k, elo, ehi) for k in range(NCH)], pr)
                (ph, rh), (pgb, rgb), (pgc, rgc), (pu, ru) = banks
                while len(pending) > 0:
                    pending.pop(0)()
                bi_ = self.rotate("mx", 2)
                (T1b, rT1), (GBb, rGB), (Ub, rU) = T1s[bi_], GBs[bi_], Us[bi_]
                PBb, rPB = PBs[bi_]
                P.add(ACT, lambda e, ph=ph, TE=TE, T1b=T1b: e.copy(out=T1b[:, 0:TE], in_=ph[:, 0:TE]), reads=[rh], writes=[rT1])
                P.add(DVE, lambda e, pgc=pgc, TE=TE, T1b=T1b: e.tensor_tensor(out=Cb[:, 0:TE], in0=pgc[:, 0:TE], in1=T1b[:, 0:TE], op=ALU.mult),
                      reads=[rgc, rT1], writes=[rC])
                P.add(ACT, lambda e, pgb=pgb, T=T, GBb=GBb: e.copy(out=GBb[:, 0:T], in_=pgb[:, 8:8 + T]), reads=[rgb], writes=[rGB])
                P.add(ACT, lambda e, pu=pu, TE=TE, Ub=Ub: e.copy(out=Ub[:, 0:TE], in_=pu[:, 0:TE]), reads=[ru], writes=[rU])
                P.add(DVE, lambda e, T=T, i=i: e.tensor_scalar(out=T2b[:, 0:T], in0=Cb[:, 7:7 + T], scalar1=VEC[:, V_CONVA + 0, i:i + 1], scalar2=None, op0=ALU.mult),
                      reads=[rC, ("VEC", 0, 1)], writes=[rT2])
                for kk in (1, 2):
                    P.add(DVE, lambda e, T=T, i=i, kk=kk: e.scalar_tensor_tensor(
                        out=T2b[:, 0:T], in0=Cb[:, 7 + kk:7 + kk + T], scalar=VEC[:, V_CONVA + kk, i:i + 1], in1=T2b[:, 0:T],
                        op0=ALU.mult, op1=ALU.add), reads=[rC, rT2, ("VEC", 0, 1)], writes=[rT2])
                P.add(DVE, lambda e, T=T, i=i, lo=lo, hi=hi, GBb=GBb: e.tensor_tensor(out=YC[:, i, lo - R1LO:hi - R1LO], in0=T2b[:, 0:T], in1=GBb[:, 0:T], op=ALU.mult),
                      reads=[rT2, rGB], writes=[rYC(i, lo, hi)])
                srcU, rsU = Ub, rU
                n = TE
                step = 1
                bufs = [(S1b, rS1), (S2b, rS2)]
                bi = 0
                while step < win:
                    n2 = n - step
                    dS, rdS = bufs[bi]
                    P.add(DVE, lambda e, srcU=srcU, dS=dS, n2=n2, step=step: e.tensor_tensor(out=dS[:, 0:n2], in0=srcU[:, 0:n2], in1=srcU[:, step:step + n2], op=ALU.add),
                          reads=[rsU], writes=[rdS])
                    srcU, rsU = dS, rdS
                    n = n2
                    step *= 2
                    bi ^= 1
                off = 8 - left
                P.add(DVE, lambda e, srcU=srcU, off=off, T=T, win=win, Ub=Ub, PBb=PBb: e.scalar_tensor_tensor(out=PBb[:, 0:T], in0=srcU[:, off:off + T], scalar=1.0 / win, in1=Ub[:, 8:8 + T],
                                                                                             op0=ALU.mult, op1=ALU.subtract),
                      reads=[rsU, rU], writes=[rPB])
                for (a, b) in ((R1LO, R1LO + 32), (R1HI - 32, R1HI)):
                    if not (lo <= a and b <= hi):
                        continue
                    ma, mb = a - 8, b + 7
                    srcK, rsK = None, None
                    nk = mb - ma
                    stepk = 1
                    kb = [(K1b, rK1), (K2b, rK2)]
                    ki = 0
                    while stepk < win:
                        n2 = nk - stepk
                        dK, rdK = kb[ki]
                        if srcK is None:
                            P.add(DVE, lambda e, dK=dK, n2=n2, stepk=stepk, ma=ma: e.tensor_tensor(out=dK[:, 0:n2], in0=self.MASK[:, ma:ma + n2], in1=self.MASK[:, ma + stepk:ma + stepk + n2], op=ALU.add),
                                  reads=[("MASK", ma, mb)], writes=[rdK])
                        else:
                            P.add(DVE, lambda e, srcK=srcK, dK=dK, n2=n2, stepk=stepk: e.tensor_tensor(out=dK[:, 0:n2], in0=srcK[:, 0:n2], in1=srcK[:, stepk:stepk + n2], op=ALU.add),
                                  reads=[rsK], writes=[rdK])
                        srcK, rsK = dK, rdK
                        nk = n2
                        stepk *= 2
                        ki ^= 1
                    P.add(DVE, lambda e, srcK=srcK, off=off: e.tensor_scalar(out=PEb[:, 0:32], in0=srcK[:, off:off + 32], scalar1=1.0, scalar2=None, op0=ALU.max),
                          reads=[rsK], writes=[rPE])
                    P.add(DVE, lambda e: e.reciprocal(out=PEb[:, 0:32], in_=PEb[:, 0:32]), reads=[rPE], writes=[rPE])
                    eo = a - elo
                    P.add(DVE, lambda e, srcU=srcU, eo=eo, left=left: e.tensor_tensor(out=PEb[:, 0:32], in0=srcU[:, eo - left:eo - left + 32], in1=PEb[:, 0:32], op=ALU.mult),
                          reads=[rsU, rPE], writes=[rPE])
                    P.add(DVE, lambda e, eo=eo, a=a, lo=lo, Ub=Ub, PBb=PBb: e.tensor_tensor(out=PBb[:, a - lo:a - lo + 32], in0=PEb[:, 0:32], in1=Ub[:, eo:eo + 32], op=ALU.subtract),
                          reads=[rPE, rU, rPB], writes=[rPB])
                def pool_tail(T=T, i=i, lo=lo, hi=hi, PBb=PBb, rPB=rPB):
                    ps, pr = self.bank()
                    P.add(PE, lambda e, ps=ps: e.matmul(ps[:, 0:T], lhsT=self.POOLW[:, i, :], rhs=PBb[:, 0:T], start=True, stop=True),
                          reads=[rPB, ("POOLW", 0, 1)], writes=[pr])
                    P.add(ACT, lambda e, ps=ps: e.activation(out=YC[:, 4 + i, lo - R1LO:hi - R1LO], in_=ps[:, 0:T], func=AF.Identity,
                                                             scale=VEC[:, V_POOL_SCALE, i:i + 1]),
                          reads=[pr, ("VEC", 0, 1)], writes=[rYC(4 + i, lo, hi)])
                pending.append(pool_tail)
        while pending:
            pending.pop(0)()
        self.linear_residual(self.kunits("w_out", None, D), YC, rYC, T0, src_off=R1LO)

    def xattn(self, L, tiles, vnorm, vnormmem, kv_done=False, kv_only=False):
        P, I = self.P, self.I
        QT = self.A[:, :].rearrange("p (c t) -> p c t", c=NCH)
        MEMFf, rMEMF = self.Bv(0, 2048)
        MEMF = MEMFf.rearrange("p (c m) -> p c m", c=NCH)
        MEMNf, _ = self.Bv(2048, 3072)
        MEMN = MEMNf.bitcast(BF16).rearrange("p (c m) -> p c m", c=NCH)
        rMEMN = lambda c0, c1: ("B", 2048 + c0 * 128, 2048 + c1 * 128)
        ESf, _ = self.Bv(3072, 4096)
        ESb = ESf.bitcast(BF16)
        rES = lambda i: ("B", 3072 + i * 256, 3072 + (i + 1) * 256)
        if not kv_done:
            P.add(SP, lambda e: e.dma_start(out=MEMF, in_=I["memT"].rearrange("(c p) m -> p c m", p=128)), writes=[rMEMF], dma="m%d" % L)
            for c in range(NCH):
                P.add(ACT, lambda e, c=c: e.activation(out=MEMN[:, c, :], in_=MEMF[:, c, :], func=AF.Square), reads=[rMEMF], writes=[rMEMN(c, c + 1)])
            ps, pr = self.bank()
            self.mm_group(ps, MEM, [(self.ONES[:, :], MEMN[:, c, :]) for c in range(NCH)], [rMEMN(0, NCH), ("ONES", 0, 1)], pr)
            r = self.rotate("rt", 2)
            RT, RS = self.RT[r], self.RS[r]
            P.add(ACT, lambda e, ps=ps, RT=RT: e.activation(out=RT[:, 0:MEM], in_=ps[:, 0:MEM], func=AF.Ln, scale=1.0 / D, bias=self.EPSV[:, 0:1]),
                  reads=[pr, ("EPSV", 0, 1)], writes=[("RT%d" % r, 0, 1)])
            P.add(ACT, lambda e, RT=RT, RS=RS: e.activation(out=RS[:, 0:MEM], in_=RT[:, 0:MEM], func=AF.Exp, scale=-0.5), reads=[("RT%d" % r, 0, 1)], writes=[("RS%d" % r, 0, 1)])
            for c in range(NCH):
                P.add(DVE, lambda e, c=c, RS=RS: e.scalar_tensor_tensor(out=MEMN[:, c, :], in0=MEMF[:, c, :], scalar=self.VEC[:, vnormmem, c:c + 1], in1=RS[:, 0:MEM],
                                                                        op0=ALU.mult, op1=ALU.mult),
                      reads=[rMEMF, ("RS%d" % r, 0, 1), ("VEC", 0, 1)], writes=[rMEMN(c, c + 1)])
            kvunits = self.kunits("xa_wkv", L, 2 * D)
            for u in range(2):
                wv, wr = self.next_unit(*kvunits[u])
                for j in range(4):
                    oc = u * 4 + j
                    ps, pr = self.bank()
                    self.mm_group(ps, MEM, [(wv[:, k, j * 128:(j + 1) * 128], MEMN[:, k, :]) for k in range(NCH)], [wr, rMEMN(0, NCH)], pr)
                    P.add(ACT, lambda e, ps=ps, oc=oc: e.copy(out=self.KT[:, oc, :], in_=ps[:, 0:MEM]), reads=[pr], writes=[("KT", oc, oc + 1)])
            for u in range(2):
                wv, wr = self.next_unit(*kvunits[2 + u])
                for mc in range(2):
                    ps, pr = self.bank()
                    self.mm_group(ps, 512, [(MEMN[:, k, mc * 128:(mc + 1) * 128], wv[:, k, :]) for k in range(NCH)], [wr, rMEMN(0, NCH)], pr)
                    P.add(DVE, lambda e, ps=ps, mc=mc, u=u: e.tensor_copy(out=self.V[:, mc, u * 512:(u + 1) * 512], in_=ps[:, 0:512]),
                          reads=[pr], writes=[("V", mc * 2 + u, mc * 2 + u + 1)])
        if kv_only:
            return
        self.rmsnorm(vnorm, tiles)
        qunits = self.kunits("xa_wq", L, D)
        ev = 0
        for u in range(2):
            wv, wr = self.next_unit(*qunits[u])
            for j in range(4):
                oc = u * 4 + j
                for (lo, hi) in tiles:
                    T = hi - lo
                    ps, pr = self.bank()
                    self.mm_group(ps, T, [(wv[:, k, j * 128:(j + 1) * 128], self.XN[:, k, lo:hi]) for k in range(NCH)],
                                  [wr] + [("XN%d" % k, lo, hi) for k in range(NCH)], pr)
                    if ev % 2 == 0:
                        P.add(ACT, lambda e, ps=ps, oc=oc, lo=lo, hi=hi, T=T: e.copy(out=QT[:, oc, lo - R1LO:hi - R1LO], in_=ps[:, 0:T]),
                              reads=[pr], writes=[rYC(oc, lo, hi)])
                    else:
                        P.add(DVE, lambda e, ps=ps, oc=oc, lo=lo, hi=hi, T=T: e.tensor_copy(out=QT[:, oc, lo - R1LO:hi - R1LO], in_=ps[:, 0:T]),
                              reads=[pr], writes=[rYC(oc, lo, hi)])
                    ev += 1
        apend = []
        for (lo, hi) in tiles:
            T = hi - lo
            for h in range(4):
                es = self.rotate("es", 2)
                ES = [ESb[:, (es * 2 + mc) * 512:(es * 2 + mc) * 512 + 512] for mc in range(2)]
                rESl = [rES(es * 2 + mc) for mc in range(2)]
                RSM, rRSM = self.Bv(4096 + es * 512, 4096 + es * 512 + 512)
                for mc in range(2):
                    ps, pr = self.bank()
                    self.mm_group(ps, T, [(self.KT[:, 2 * h + hc, mc * 128:(mc + 1) * 128], QT[:, 2 * h + hc, lo - R1LO:hi - R1LO]) for hc in range(2)],
                                  [("KT", 2 * h, 2 * h + 2), rYC(2 * h, lo, hi), rYC(2 * h + 1, lo, hi)], pr)
                    P.add(ACT, lambda e, ps=ps, mc=mc, T=T, ES=ES: e.activation(out=ES[mc][:, 0:T], in_=ps[:, 0:T], func=AF.Exp, scale=1.0 / 16.0),
                          reads=[pr], writes=[rESl[mc]])
                while apend:
                    apend.pop(0)()

                def attn_tail(T=T, ES=ES, rESl=rESl, RSM=RSM, rRSM=rRSM, h=h, lo=lo, hi=hi):
                    ps, pr = self.bank()
                    self.mm_group(ps, T, [(self.ONES[:, :], ES[mc][:, 0:T]) for mc in range(2)], [rESl[0], rESl[1], ("ONES", 0, 1)], pr)
                    P.add(ACT, lambda e, ps=ps: e.activation(out=RSM[:, 0:T], in_=ps[:, 0:T], func=AF.Ln), reads=[pr], writes=[rRSM])
                    P.add(ACT, lambda e: e.activation(out=RSM[:, 0:T], in_=RSM[:, 0:T], func=AF.Exp, scale=-1.0), reads=[rRSM], writes=[rRSM])
                    for hc in range(2):
                        oc = 2 * h + hc
                        ps, pr = self.bank()
                        self.mm_group(ps, T, [(self.V[:, mc, oc * 128:(oc + 1) * 128], ES[mc][:, 0:T]) for mc in range(2)],
                                      [("V", 0, 4), rESl[0], rESl[1]], pr)
                        P.add(DVE, lambda e, ps=ps, oc=oc: e.tensor_tensor(out=QT[:, oc, lo - R1LO:hi - R1LO], in0=ps[:, 0:T], in1=RSM[:, 0:T], op=ALU.mult),
                              reads=[pr, rRSM], writes=[rYC(oc, lo, hi)])
                apend.append(attn_tail)
        while apend:
            apend.pop(0)()
        self.linear_residual(self.kunits("xa_wo", L, D), QT, rYC, tiles, src_off=R1LO)

    def swiglu(self, key, sub, dff, tiles, crow=None, crowr=None, mid_hook=None):
        P = self.P
        nfc = dff // 128
        HT = [self.A[:, b * 4 * NR1:(b + 1) * 4 * NR1].rearrange("p (j t) -> p j t", j=4) for b in range(2)]
        SGf, _ = self.Cv(0, 512)
        SGb = SGf.bitcast(BF16)
        rSG = lambda s: ("C", s * 256, (s + 1) * 256)
        t0 = tiles[0][0]
        g0 = 0
        while g0 < nfc:
            ng = min(4, nfc - g0)
            hb = self.rotate("ht", 2)
            wg, wgr = self.next_unit(("k", ng * 128), (key, sub, g0 * 128, ng * 128))
            wu, wur = self.next_unit(("k", ng * 128), (key, sub, dff + g0 * 128, ng * 128), live_prev=1)
            for j in range(ng):
                for (lo, hi) in tiles:
                    T = hi - lo
                    pg, pgr = self.bank()
                    pu, pur = self.bank()
                    for (ps, pr, wv, wr) in ((pg, pgr, wg, wgr), (pu, pur, wu, wur)):
                        self.mm_group(ps, T, [(wv[:, k, j * 128:(j + 1) * 128], self.XN[:, k, lo:hi]) for k in range(NCH)],
                                      [wr] + [("XN%d" % k, lo, hi) for k in range(NCH)], pr)
                    s = self.rotate("sg", 2)
                    SG = SGb[:, s * 512:s * 512 + 512]
                    P.add(ACT, lambda e, pg=pg, SG=SG, T=T: e.activation(out=SG[:, 0:T], in_=pg[:, 0:T], func=AF.Silu), reads=[pgr], writes=[rSG(s)])
                    hdst = HT[hb][:, j, lo - t0:hi - t0]
                    hr = rHT(hb, j, lo, hi, t0)
                    if crow is None:
                        P.add(DVE, lambda e, pu=pu, SG=SG, T=T, hdst=hdst: e.tensor_tensor(out=hdst, in0=pu[:, 0:T], in1=SG[:, 0:T], op=ALU.mult),
                              reads=[pur, rSG(s)], writes=[hr])
                    else:
                        TU, rTU = self.Cv(512 + s * 512, 1024 + s * 512)
                        P.add(DVE, lambda e, pu=pu, TU=TU, T=T, lo=lo, hi=hi: e.tensor_tensor(out=TU[:, 0:T], in0=pu[:, 0:T], in1=crow[:, lo - OWN0:hi - OWN0], op=ALU.mult),
                              reads=[pur, crowr(lo, hi)], writes=[rTU])
                        P.add(DVE, lambda e, SG=SG, TU=TU, T=T, hdst=hdst: e.tensor_tensor(out=hdst, in0=TU[:, 0:T], in1=SG[:, 0:T], op=ALU.mult),
                              reads=[rTU, rSG(s)], writes=[hr])
            if mid_hook is not None and g0 == 12:
                mid_hook()
            dkey = {"ffn_gu": "ffn_down", "moe_gu": "moe_down"}[key]
            wd, wdr = self.next_unit(("d", ng), (dkey, sub, g0, ng))
            for oc in range(NCH):
                for (lo, hi) in tiles:
                    T = hi - lo
                    ps, pr = self.bank()
                    self.mm_group(ps, T, [(wd[:, j, oc * 128:(oc + 1) * 128], HT[hb][:, j, lo - t0:hi - t0]) for j in range(ng)],
                                  [wdr] + [rHT(hb, j, lo, hi, t0) for j in range(ng)], pr)
                    P.add(DVE, lambda e, ps=ps, oc=oc, lo=lo, hi=hi, T=T: e.tensor_tensor(out=self.H[:, oc, lo:hi], in0=ps[:, 0:T], in1=self.H[:, oc, lo:hi], op=ALU.add),
                          reads=[pr, ("H%d" % oc, lo, hi)], writes=[("H%d" % oc, lo, hi)])
            g0 += ng

    def conformer(self):
        P = self.P
        VEC = self.VEC
        CV = self.A[:, 0:NCH * OWN].rearrange("p (c t) -> p c t", c=NCH)
        W = 448
        SIGs = [self.Cv(W * i, W * (i + 1)) for i in range(2)]
        HGfs = [self.Cv(W * (2 + i), W * (3 + i)) for i in range(2)]
        HGbf, _ = self.Cv(W * 4, W * 5)
        HGbb = HGbf.bitcast(BF16)
        rHGb = lambda i: ("C", W * 4 + i * 224, W * 4 + (i + 1) * 224)
        DGW = 31 * 64
        DGs = []
        for i in range(2):
            f, r = self.Bv(i * DGW, (i + 1) * DGW)
            DGs.append((f.bitcast(BF16).rearrange("p (k n) -> p k n", k=31), r))
        TN = [self.Bv(1024 + 512 * i, 1536 + 512 * i) for i in range(2)]
        self.rmsnorm(V_OD_NORM_MIX, T0)
        units = self.kunits("pw1", None, 2048)
        pending = []
        for u in range(4):
            wv, wr = self.next_unit(*units[u])
            for q in range(2):
                c = 2 * u + q
                dgi = self.rotate("dg31", 2)
                DG, rDG = DGs[dgi]
                for kk in range(31):
                    P.add(DVE, lambda e, kk=kk, c=c, DG=DG: e.tensor_scalar(out=DG[:, kk, :], in0=self.IDENT[:, :], scalar1=VEC[:, V_DW + kk, c:c + 1], scalar2=None, op0=ALU.mult),
                          reads=[("IDENT", 0, 1), ("VEC", 0, 1)], writes=[(rDG[0], rDG[1] + kk * 64, rDG[1] + (kk + 1) * 64)])
                for (lo, hi) in TC:
                    T = hi - lo
                    elo, ehi = lo - 15, hi + 15
                    TE = ehi - elo
                    pa, par = self.bank()
                    pg, pgr = self.bank()
                    for (ps, pr, col) in ((pa, par, q), (pg, pgr, 2 + q)):
                        self.mm_group(ps, TE, [(wv[:, k, col * 128:(col + 1) * 128], self.XN[:, k, elo:ehi]) for k in range(NCH)],
                                      [wr] + [("XN%d" % k, elo, ehi) for k in range(NCH)], pr)
                    while pending:
                        pending.pop(0)()
                    si = self.rotate("hg", 2)
                    (SIG, rSIG), (HGf, rHGf) = SIGs[si], HGfs[si]
                    HGb = HGbb[:, si * 448:(si + 1) * 448]
                    P.add(ACT, lambda e, pg=pg, c=c, TE=TE, SIG=SIG: e.activation(out=SIG[:, 0:TE], in_=pg[:, 0:TE], func=AF.Sigmoid, bias=VEC[:, V_PW1_BG, c:c + 1]),
                          reads=[pgr, ("VEC", 0, 1)], writes=[rSIG])
                    P.add(DVE, lambda e, pa=pa, c=c, TE=TE, HGf=HGf, SIG=SIG: e.scalar_tensor_tensor(out=HGf[:, 0:TE], in0=pa[:, 0:TE], scalar=VEC[:, V_PW1_BA, c:c + 1], in1=SIG[:, 0:TE],
                                                                                            op0=ALU.add, op1=ALU.mult),
                          reads=[par, rSIG, ("VEC", 0, 1)], writes=[rHGf])
                    P.add(DVE, lambda e, TE=TE, HGf=HGf, HGb=HGb, elo=elo, ehi=ehi: e.tensor_tensor(out=HGb[:, 0:TE], in0=HGf[:, 0:TE], in1=self.MASK[:, elo:ehi], op=ALU.mult),
                          reads=[rHGf, ("MASK", elo, ehi)], writes=[rHGb(si)])
                    def conv_tail(T=T, DG=DG, rDG=rDG, HGb=HGb, si=si, c=c, lo=lo, hi=hi):
                        ps, pr = self.bank()
                        self.mm_group(ps, T, [(DG[:, kk, :], HGb[:, kk:kk + T]) for kk in range(31)], [rDG, rHGb(si)], pr)
                        P.add(ACT, lambda e, ps=ps: e.activation(out=CV[:, c, lo - OWN0:hi - OWN0], in_=ps[:, 0:T], func=AF.Identity, bias=VEC[:, V_DW_B, c:c + 1]),
                              reads=[pr, ("VEC", 0, 1)], writes=[rCV(c, lo, hi)])
                    pending.append(conv_tail)
        while pending:
            pending.pop(0)()
        pw2u = self.kunits("pw2", None, D)
        pw2w = [self.next_unit(*pw2u[0]), self.next_unit(*pw2u[1], live_prev=1)]
        MUs = [self.Bv(0, 512), self.Bv(2048, 2560)]
        VARs = [self.Bv(512, 1024), self.Bv(2560, 3072)]
        st = {}

        def stage_a1(ti):
            lo, hi = TC[ti]
            T = hi - lo
            for c in range(NCH):
                P.add(ACT, lambda e, c=c: e.activation(out=self.XN[:, c, lo:hi], in_=CV[:, c, lo - OWN0:hi - OWN0], func=AF.Square),
                      reads=[rCV(c, lo, hi)], writes=[("XN%d" % c, lo, hi)])
            p1, p1r = self.bank()
            p2, p2r = self.bank()
            self.mm_group(p1, T, [(self.ONES[:, :], CV[:, c, lo - OWN0:hi - OWN0]) for c in range(NCH)], [rCV(c, lo, hi) for c in range(NCH)] + [("ONES", 0, 1)], p1r)
            self.mm_group(p2, T, [(self.ONES[:, :], self.XN[:, c, lo:hi]) for c in range(NCH)], [("XN%d" % c, lo, hi) for c in range(NCH)] + [("ONES", 0, 1)], p2r)
            r = self.rotate("rt", 2)
            st[ti] = (p1, p1r, p2, p2r, r)

        def stage_a2(ti):
            lo, hi = TC[ti]
            T = hi - lo
            p1, p1r, p2, p2r, r = st[ti]
            (MU, rMU), (VAR, rVAR) = MUs[ti % 2], VARs[ti % 2]
            RT, RS = self.RT[r], self.RS[r]
            P.add(ACT, lambda e: e.activation(out=MU[:, 0:T], in_=p1[:, 0:T], func=AF.Identity, scale=1.0 / D), reads=[p1r], writes=[rMU])
            P.add(DVE, lambda e: e.tensor_tensor(out=VAR[:, 0:T], in0=MU[:, 0:T], in1=MU[:, 0:T], op=ALU.mult), reads=[rMU], writes=[rVAR])
            P.add(DVE, lambda e: e.scalar_tensor_tensor(out=VAR[:, 0:T], in0=p2[:, 0:T], scalar=1.0 / D, in1=VAR[:, 0:T], op0=ALU.mult, op1=ALU.subtract),
                  reads=[p2r, rVAR], writes=[rVAR])
            P.add(DVE, lambda e: e.tensor_scalar(out=VAR[:, 0:T], in0=VAR[:, 0:T], scalar1=0.0, scalar2=None, op0=ALU.max),
                  reads=[rVAR], writes=[rVAR])
            P.add(ACT, lambda e: e.activation(out=RT[:, 0:T], in_=VAR[:, 0:T], func=AF.Ln, bias=self.EPSV[:, 0:1]),
                  reads=[rVAR, ("EPSV", 0, 1)], writes=[("RT%d" % r, 0, 1)])
            P.add(ACT, lambda e: e.activation(out=RS[:, 0:T], in_=RT[:, 0:T], func=AF.Exp, scale=-0.5), reads=[("RT%d" % r, 0, 1)], writes=[("RS%d" % r, 0, 1)])

        def stage_b(ti):
            lo, hi = TC[ti]
            T = hi - lo
            r = st[ti][4]
            (MU, rMU) = MUs[ti % 2]
            RS = self.RS[r]
            for c in range(NCH):
                tn = self.rotate("tn", 2)
                TNb, rTN = TN[tn]
                P.add(DVE, lambda e, c=c, TNb=TNb: e.tensor_tensor(out=TNb[:, 0:T], in0=CV[:, c, lo - OWN0:hi - OWN0], in1=MU[:, 0:T], op=ALU.subtract),
                      reads=[rCV(c, lo, hi), rMU], writes=[rTN])
                P.add(DVE, lambda e, TNb=TNb: e.tensor_tensor(out=TNb[:, 0:T], in0=TNb[:, 0:T], in1=RS[:, 0:T], op=ALU.mult),
                      reads=[rTN, ("RS%d" % r, 0, 1)], writes=[rTN])
                P.add(ACT, lambda e, c=c, TNb=TNb: e.activation(out=self.XN[:, c, lo:hi], in_=TNb[:, 0:T], func=AF.Silu,
                                                                scale=VEC[:, V_LN_G, c:c + 1], bias=VEC[:, V_LN_B, c:c + 1]),
                      reads=[rTN, ("VEC", 0, 1)], writes=[("XN%d" % c, lo, hi)])

        def stage_c(ti):
            lo, hi = TC[ti]
            T = hi - lo
            for u2 in range(2):
                wv2, wr2 = pw2w[u2]
                for j in range(4):
                    oc = u2 * 4 + j
                    ps, pr = self.bank()
                    self.mm_group(ps, T, [(wv2[:, k, j * 128:(j + 1) * 128], self.XN[:, k, lo:hi]) for k in range(NCH)],
                                  [wr2] + [("XN%d" % k, lo, hi) for k in range(NCH)], pr)
                    P.add(DVE, lambda e, ps=ps, oc=oc: e.scalar_tensor_tensor(
                        out=self.H[:, oc, lo:hi], in0=ps[:, 0:T], scalar=VEC[:, V_PW2_B, oc:oc + 1], in1=self.H[:, oc, lo:hi],
                        op0=ALU.add, op1=ALU.add),
                        reads=[pr, ("H%d" % oc, lo, hi), ("VEC", 0, 1)], writes=[("H%d" % oc, lo, hi)])

        nt = len(TC)
        stage_a1(0)
        stage_a2(0)
        for ti in range(nt):
            if ti + 1 < nt:
                stage_a1(ti + 1)
            stage_b(ti)
            if ti + 1 < nt:
                stage_a2(ti + 1)
            stage_c(ti)

    def moe(self):
        P, I = self.P, self.I
        VEC = self.VEC
        RSTDROW, _ = self.Bv(0, OWN)
        CROW, _ = self.Bv(OWN, 2 * OWN)
        rRSTD = lambda lo, hi: ("B", lo - OWN0, hi - OWN0)
        rCROW = lambda lo, hi: ("B", OWN + lo - OWN0, OWN + hi - OWN0)
        GRf, rGR = self.Cv(1536, 1600)
        GR = GRf.rearrange("p (c e) -> p c e", c=NCH)
        RTRf, rRTR = self.Cv(1600, 1664)
        RTR = RTRf.rearrange("p (c e) -> p c e", c=NCH)
        CBf, _ = self.Cv(1664, 1792)
        CBALL = CBf.rearrange("p (t e) -> p t e", t=16)
        rCB = lambda tt: ("C", 1664 + tt * 8, 1672 + tt * 8)
        (Lb, rL), (EQ1, rEQ1), (L2, rL2), (EQ2, rEQ2) = [self.Cv(1792 + 8 * i, 1800 + 8 * i) for i in range(4)]
        (M1, rM1), (M2, rM2), (DM, rDM), (EE, rEE), (P1, rP1), (P2, rP2), (RTOK, rRTOK) = [self.Cv(1824 + i, 1825 + i) for i in range(7)]
        DIAG = [self.Cv(1840 + 128 * i, 1968 + 128 * i) for i in range(2)]
        self.rmsnorm(V_NORM_MOE, T1, save_rstd=True)
        P.add(SP, lambda e: e.dma_start(out=RTR, in_=I["router"].rearrange("(c p) e -> p c e", p=128)), writes=[rRTR], dma="rt")
        for c in range(NCH):
            P.add(DVE, lambda e, c=c: e.tensor_scalar(out=GR[:, c, :], in0=RTR[:, c, :], scalar1=VEC[:, V_NORM_MOE, c:c + 1], scalar2=None, op0=ALU.mult),
                  reads=[rRTR, ("VEC", 0, 1)], writes=[("C", 1536 + 8 * c, 1544 + 8 * c)])
        for tt in range(16):
            lo = OWN0 + 128 * tt
            hi = lo + 128
            ps, pr = self.bank()
            self.mm_group(ps, NEXP, [(self.H[:, c, lo:hi], GR[:, c, :]) for c in range(NCH)], [("H%d" % c, lo, hi) for c in range(NCH)] + [rGR], pr)
            ps2, pr2 = self.bank()
            P.add(PE, lambda e, ps2=ps2, lo=lo, hi=hi: e.matmul(ps2[:, 0:1], lhsT=RSTDROW[0:1, lo - OWN0:hi - OWN0], rhs=self.ONESF[0:1, 0:1], start=True, stop=True),
                  reads=[rRSTD(lo, hi), ("ONESF", 0, 1)], writes=[pr2])
            P.add(ACT, lambda e, ps=ps: e.copy(out=Lb, in_=ps[:, 0:NEXP]), reads=[pr], writes=[rL])
            P.add(ACT, lambda e, ps2=ps2: e.copy(out=RTOK, in_=ps2[:, 0:1]), reads=[pr2], writes=[rRTOK])
            P.add(DVE, lambda e: e.reduce_max(out=M1, in_=Lb, axis=AX.X), reads=[rL], writes=[rM1])
            P.add(DVE, lambda e: e.tensor_scalar(out=EQ1, in0=Lb, scalar1=M1, scalar2=None, op0=ALU.is_equal), reads=[rL, rM1], writes=[rEQ1])
            P.add(DVE, lambda e: e.scalar_tensor_tensor(out=L2, in0=EQ1, scalar=-1.0e30, in1=Lb, op0=ALU.mult, op1=ALU.add), reads=[rEQ1, rL], writes=[rL2])
            P.add(DVE, lambda e: e.reduce_max(out=M2, in_=L2, axis=AX.X), reads=[rL2], writes=[rM2])
            P.add(DVE, lambda e: e.tensor_scalar(out=EQ2, in0=L2, scalar1=M2, scalar2=None, op0=ALU.is_equal), reads=[rL2, rM2], writes=[rEQ2])
            P.add(DVE, lambda e: e.tensor_tensor(out=DM, in0=M2, in1=M1, op=ALU.subtract), reads=[rM1, rM2], writes=[rDM])
            P.add(ACT, lambda e: e.activation(out=EE, in_=DM, func=AF.Exp, scale=RTOK), reads=[rDM, rRTOK], writes=[rEE])
            P.add(DVE, lambda e: e.tensor_scalar(out=P1, in0=EE, scalar1=1.0, scalar2=None, op0=ALU.add), reads=[rEE], writes=[rP1])
            P.add(DVE, lambda e: e.reciprocal(out=P1, in_=P1), reads=[rP1], writes=[rP1])
            P.add(DVE, lambda e: e.tensor_tensor(out=P2, in0=EE, in1=P1, op=ALU.mult), reads=[rEE, rP1], writes=[rP2])
            P.add(DVE, lambda e, tt=tt: e.tensor_scalar(out=CBALL[:, tt, :], in0=EQ1, scalar1=P1, scalar2=None, op0=ALU.mult),
                  reads=[rEQ1, rP1], writes=[rCB(tt)])
            P.add(DVE, lambda e, tt=tt: e.scalar_tensor_tensor(out=CBALL[:, tt, :], in0=EQ2, scalar=P2, in1=CBALL[:, tt, :], op0=ALU.mult, op1=ALU.add),
                  reads=[rEQ2, rP2, rCB(tt)], writes=[rCB(tt)])
        CROWS = [(CROW, rCROW), (RSTDROW, rRSTD)]

        def make_crow(ex):
            crow, crowr = CROWS[ex % 2]
            for tt in range(16):
                lo = OWN0 + 128 * tt
                hi = lo + 128
                dg = self.rotate("dg", 2)
                DG, rDG = DIAG[dg]
                P.add(DVE, lambda e, tt=tt, DG=DG: e.tensor_scalar(out=DG, in0=self.IDENT[:, :], scalar1=CBALL[:, tt, ex:ex + 1], scalar2=None, op0=ALU.mult),
                      reads=[("IDENT", 0, 1), rCB(tt)], writes=[rDG])
                ps, pr = self.bank()
                P.add(PE, lambda e, ps=ps, DG=DG: e.matmul(ps[:, 0:128], lhsT=self.ONESF[:, :], rhs=DG, start=True, stop=True),
                      reads=[rDG, ("ONESF", 0, 1)], writes=[pr])
                P.add(ACT, lambda e, ps=ps, lo=lo, hi=hi: e.copy(out=crow[:, lo - OWN0:hi - OWN0], in_=ps[:, 0:128]), reads=[pr], writes=[crowr(lo, hi)])

        make_crow(0)
        for ex in range(NEXP):
            crow, crowr = CROWS[ex % 2]
            hook = (lambda ex=ex: make_crow(ex + 1)) if ex + 1 < NEXP else None
            self.swiglu("moe_gu", ex, DFFE, T1, crow=crow, crowr=crowr, mid_hook=hook)

    def layer0(self):
        self.rmsnorm(V_EV_NORM_MIX, TN0)
        self.mixer0()
        if DEBUG_STOP == "mix":
            return
        self.xattn(0, T0, V_XA_NORM0, V_XA_NORMMEM0)
        if DEBUG_STOP == "xa0":
            return
        self.rmsnorm(V_EV_NORM_FFN, T0)
        hook = None
        if self.mode == "FUSED":
            hook = lambda: self.xattn(1, T1, V_XA_NORM1, V_XA_NORMMEM1, kv_only=True)
        self.swiglu("ffn_gu", None, DFF, T0, mid_hook=hook)

    def layer1(self):
        self.conformer()
        if DEBUG_STOP == "conf":
            return
        self.xattn(1, T1, V_XA_NORM1, V_XA_NORMMEM1, kv_done=(self.mode == "FUSED"))
        if DEBUG_STOP == "xa1":
            return
        self.moe()
        self.rmsnorm(V_FINAL, T1, out_f32_inplace=True)


def build_program(mode):
    b1 = Builder(mode)
    b1.build()
    b2 = Builder(mode, unit_plan=b1.plan)
    nc = b2.build()
    return nc, b2


def _feat(v):
    return np.ascontiguousarray(np.asarray(v, np.float32).reshape(8, 128).T)


def _feat_small(v, n):
    out = np.zeros((128, 8), np.float32)
    out[:, :n] = np.asarray(v, np.float32).reshape(n, 128).T
    return out


def _prep_common(inp):
    vecs = np.zeros((128, NVEC, 8), np.float32)
    vecs[:, V_EV_NORM_MIX] = _feat(inp["ev_norm_mix"][0])
    vecs[:, V_EV_NORM_FFN] = _feat(inp["ev_norm_ffn"][0])
    vecs[:, V_XA_NORM0] = _feat(inp["xa_norm"][0])
    vecs[:, V_XA_NORMMEM0] = _feat(inp["xa_norm_mem"][0])
    vecs[:, V_XA_NORM1] = _feat(inp["xa_norm"][1])
    vecs[:, V_XA_NORMMEM1] = _feat(inp["xa_norm_mem"][1])
    vecs[:, V_OD_NORM_MIX] = _feat(inp["od_norm_mix"][0])
    vecs[:, V_PW1_BA] = _feat(inp["od_pw1_b"][0][:1024])
    vecs[:, V_PW1_BG] = _feat(inp["od_pw1_b"][0][1024:])
    vecs[:, V_DW_B] = _feat(inp["od_dw_b"][0])
    vecs[:, V_LN_G] = _feat(inp["od_ln_g"][0])
    vecs[:, V_LN_B] = _feat(inp["od_ln_b"][0])
    vecs[:, V_PW2_B] = _feat(inp["od_pw2_b"][0])
    vecs[:, V_NORM_MOE] = _feat(inp["od_norm_moe"][0])
    vecs[:, V_FINAL] = _feat(inp["final_norm"])
    vecs[:, V_POOL_SCALE] = _feat_small(inp["ev_pool_scale"][0], 4)
    for k in range(3):
        vecs[:, V_CONVA + k] = _feat_small(inp["ev_conv_a"][0][k], 4)
    for k in range(31):
        vecs[:, V_DW + k] = _feat(inp["od_dw_w"][0][k])
    perm_in = np.concatenate([np.concatenate([g * 512 + i * 128 + np.arange(128) for g in range(4)]) for i in range(4)])
    perm_pw1 = np.concatenate([np.concatenate([(2 * j) * 128 + np.arange(256), 1024 + (2 * j) * 128 + np.arange(256)]) for j in range(4)])
    f = lambda a: np.ascontiguousarray(np.asarray(a, np.float32))
    common = {
        "vecs": vecs,
        "ident": np.eye(128, dtype=np.float32),
        "xa_wq": f(inp["xa_wq"]), "xa_wkv": f(inp["xa_wkv"]), "xa_wo": f(inp["xa_wo"]),
        "w_in": f(np.asarray(inp["ev_w_in"][0])[:, perm_in]),
        "pool_w": f(inp["ev_pool_w"][0]),
        "w_out": f(inp["ev_w_out"][0]),
        "ffn_gu": f(inp["ev_ffn_gu"][0]),
        "ffn_down": f(inp["ev_ffn_down"][0]),
        "pw1": f(np.asarray(inp["od_pw1_w"][0])[:, perm_pw1]),
        "pw2": f(inp["od_pw2_w"][0]),
        "router": f(inp["od_router"][0]),
        "moe_gu": f(inp["od_moe_gu"][0]),
        "moe_down": f(inp["od_moe_down"][0]),
    }
    return common


def _prep_core(inp, core):
    b, half = core // 2, core % 2
    s0 = half * OWN
    x = np.asarray(inp["x"], np.float32)
    xT = np.zeros((D, N0), np.float32)
    mask = np.zeros((N0,), np.float32)
    g0, g1 = s0 - HALO, s0 + OWN + HALO
    v0, v1 = max(g0, 0), min(g1, SEQ)
    xT[:, v0 - g0:v1 - g0] = x[b, v0:v1, :].T
    mask[v0 - g0:v1 - g0] = 1.0
    memT = np.ascontiguousarray(np.asarray(inp["mem"], np.float32)[b].T)
    return {"xT": xT, "maskb": np.ascontiguousarray(np.broadcast_to(mask, (128, N0))), "memT": memT}


_L0_KEYS = ["xT", "maskb", "memT", "vecs", "ident", "xa_wq", "xa_wkv", "xa_wo", "w_in", "pool_w", "w_out", "ffn_gu", "ffn_down"]
_L1_KEYS = ["h1T", "maskb", "memT", "vecs", "ident", "xa_wq", "xa_wkv", "xa_wo", "pw1", "pw2", "router", "moe_gu", "moe_down"]
_FUSED_KEYS = ["xT", "maskb", "memT", "vecs", "ident", "xa_wq", "xa_wkv", "xa_wo", "w_in", "pool_w", "w_out", "ffn_gu", "ffn_down",
               "pw1", "pw2", "router", "moe_gu", "moe_down"]

_CACHE = {}
FUSED = True


def _get_prog(mode):
    if mode not in _CACHE:
        _CACHE[mode] = build_program(mode)[0]
    return _CACHE[mode]


def kernel(**inputs):
    common = _prep_common(inputs)
    cores = [dict(common, **_prep_core(inputs, i)) for i in range(8)]
    out = np.zeros((4, SEQ, D), np.float32)
    if FUSED:
        nc = _get_prog("FUSED")
        res = run_bass_kernel_spmd(nc, [{k: c[k] for k in _FUSED_KEYS} for c in cores], core_ids=list(range(8)))
        outs = [r["outT"] for r in res.results]
    else:
        nc0 = _get_prog("L0")
        res0 = run_bass_kernel_spmd(nc0, [{k: c[k] for k in _L0_KEYS} for c in cores], core_ids=list(range(8)))
        for i in range(8):
            cores[i]["h1T"] = np.asarray(res0.results[i]["h1o"])
        nc1 = _get_prog("L1")
        res1 = run_bass_kernel_spmd(nc1, [{k: c[k] for k in _L1_KEYS} for c in cores], core_ids=list(range(8)))
        outs = [r["outT"] for r in res1.results]
    for i in range(8):
        b, half = i // 2, i % 2
        out[b, half * OWN:(half + 1) * OWN, :] = np.asarray(outs[i]).T
    return out
```

```python
from contextlib import ExitStack
import numpy as np
import concourse.bass as bass
import concourse.mybir as mybir
from concourse.bass_utils import run_bass_kernel_spmd

F32 = mybir.dt.float32
BF16 = mybir.dt.bfloat16
AF = mybir.ActivationFunctionType
ALU = mybir.AluOpType
AX = mybir.AxisListType

PE, ACT, DVE, POOL, SP = "tensor", "scalar", "vector", "gpsimd", "sync"
ENGINES = (PE, ACT, DVE, POOL, SP)

D = 1024
NCH = 8
SEQ = 4096
OWN = 2048
HALO = 24
N0 = OWN + 2 * HALO
OWN0 = HALO
R1LO, R1HI = 8, 2088
NR1 = R1HI - R1LO
T0 = [(R1LO + 416 * i, R1LO + 416 * (i + 1)) for i in range(5)]
T1 = [(OWN0 + 512 * i, OWN0 + 512 * (i + 1)) for i in range(4)]
TC = [(OWN0 + 410 * i, min(OWN0 + 410 * (i + 1), OWN0 + OWN)) for i in range(5)]
TN0 = [(0, 432), (432, 848), (848, 1264), (1264, 1680), (1680, 2096)]
MEM = 256
DFF = 2816
DFFE = 3584
NEXP = 8
EPS = 1e-6

V_EV_NORM_MIX, V_EV_NORM_FFN, V_XA_NORM0, V_XA_NORMMEM0, V_XA_NORM1, V_XA_NORMMEM1 = 0, 1, 2, 3, 4, 5
V_OD_NORM_MIX, V_PW1_BA, V_PW1_BG, V_DW_B, V_LN_G, V_LN_B, V_PW2_B, V_NORM_MOE, V_FINAL = 6, 7, 8, 9, 10, 11, 12, 13, 14
V_POOL_SCALE = 15
V_CONVA = 16
V_DW = 19
NVEC = 50


class Op:
    __slots__ = ("eng", "fn", "deps", "dmakey", "dmaval", "signal", "val", "idx", "dmawaits")

    def __init__(self, eng, fn, dmakey, idx):
        self.eng = eng
        self.fn = fn
        self.deps = {}
        self.dmawaits = {}
        self.dmakey = dmakey
        self.dmaval = 0
        self.signal = False
        self.val = 0
        self.idx = idx


class Prog:
    def __init__(self, dry=False):
        self.ops = {e: [] for e in ENGINES}
        self.bufs = {}
        self.dma_count = {}
        self.n = 0
        self.dry = dry

    def add(self, eng, fn, reads=(), writes=(), dma=None):
        if self.dry:
            return None
        op = Op(eng, fn, dma, self.n)
        self.n += 1
        deps = []
        for (name, lo, hi) in reads:
            b = self.bufs.get(name)
            if b is None:
                b = self.bufs[name] = ([], [])
            for (l, h, o) in b[0]:
                if l < hi and lo < h:
                    deps.append(o)
        for (name, lo, hi) in writes:
            b = self.bufs.get(name)
            if b is None:
                b = self.bufs[name] = ([], [])
            for (l, h, o) in b[0]:
                if l < hi and lo < h:
                    deps.append(o)
            for (l, h, o) in b[1]:
                if l < hi and lo < h:
                    deps.append(o)
        for o in deps:
            if o.dmakey is not None:
                v = 16 * self.dma_count[o.dmakey]
                if op.dmawaits.get(o.dmakey, 0) < v:
                    op.dmawaits[o.dmakey] = v
            else:
                if o.eng == PE and eng == PE:
                    continue
                cur = op.deps.get(o.eng)
                if cur is None or cur.idx < o.idx:
                    op.deps[o.eng] = o
        for o in op.deps.values():
            o.signal = True
        isdma = dma is not None
        for (name, lo, hi) in reads:
            b = self.bufs[name]
            if not isdma:
                b[1][:] = [(l, h, o) for (l, h, o) in b[1]
                           if not (o.eng == eng and o.dmakey is None and lo <= l and h <= hi)]
            b[1].append((lo, hi, op))
        for (name, lo, hi) in writes:
            b = self.bufs[name]
            b[0][:] = [(l, h, o) for (l, h, o) in b[0] if not (lo <= l and h <= hi)]
            b[1][:] = [(l, h, o) for (l, h, o) in b[1] if not (lo <= l and h <= hi)]
            b[0].append((lo, hi, op))
        if isdma:
            self.dma_count[dma] = self.dma_count.get(dma, 0) + 1
            op.dmaval = 16 * self.dma_count[dma]
        self.ops[eng].append(op)
        return op

    def emit(self, nc, final_waits=()):
        for e in ENGINES:
            c = 0
            for op in self.ops[e]:
                if op.dmakey is None and op.signal:
                    c += 1
                    op.val = c
        stats = {e: len(self.ops[e]) for e in ENGINES}
        with ExitStack() as st:
            esem = {e: st.enter_context(nc.semaphore("s_" + e)) for e in ENGINES}
            dsem = {k: st.enter_context(nc.semaphore("d_" + k)) for k in self.dma_count}
            block = st.enter_context(nc.Block())
            nwaits = {e: 0 for e in ENGINES}

            def make_body(e):
                def body(eng):
                    waited = {}
                    for op in self.ops[e]:
                        for pe_, o in op.deps.items():
                            key = ("e", pe_)
                            if waited.get(key, 0) < o.val:
                                eng.wait_ge(esem[pe_], o.val)
                                waited[key] = o.val
                                nwaits[e] += 1
                        for k, v in op.dmawaits.items():
                            key = ("d", k)
                            if waited.get(key, 0) < v:
                                eng.wait_ge(dsem[k], v)
                                waited[key] = v
                                nwaits[e] += 1
                        ins = op.fn(eng)
                        if op.dmakey is not None:
                            ins.then_inc(dsem[op.dmakey], 16)
                        elif op.signal:
                            ins.then_inc(esem[e], 1)
                    if e == SP:
                        for k in final_waits:
                            eng.wait_ge(dsem[k], 16 * self.dma_count[k])
                return body

            for e in ENGINES:
                getattr(block, e)(make_body(e))
        stats["waits"] = nwaits
        return stats


NSLOT = 3
DEBUG_STOP = None
CW = 2304
BW = 5120


def rYC(c, lo, hi):
    return ("A", c * NR1 + lo - R1LO, c * NR1 + hi - R1LO)


def rHT(b, j, lo, hi, t0):
    base = (b * 4 + j) * NR1
    return ("A", base + lo - t0, base + hi - t0)


def rCV(c, lo, hi):
    return ("A", c * OWN + lo - OWN0, c * OWN + hi - OWN0)


class Builder:
    def __init__(self, mode, unit_plan=None):
        assert mode in ("L0", "L1", "FUSED")
        self.mode = mode
        self.record = unit_plan is None
        self.plan = [] if unit_plan is None else unit_plan
        self.P = Prog(dry=self.record)
        self.uidx = 0
        self.issued = 0
        self.bank_i = 0
        self.rot = {}

    def bank(self):
        b = self.bank_i % 8
        self.bank_i += 1
        st = self.P.bufs.get("ps%d" % b)
        if st is not None and st[0] and not st[1]:
            raise RuntimeError("PSUM bank %d re-allocated before its last result was read (emission-order bug)" % b)
        return self.PS[b], ("ps%d" % b, 0, 1)

    def rotate(self, key, n):
        i = self.rot.get(key, 0)
        self.rot[key] = i + 1
        return i % n

    def Bv(self, lo, hi):
        return self.B[:, lo:hi], ("B", lo, hi)

    def Cv(self, lo, hi):
        return self.C[:, lo:hi], ("C", lo, hi)

    def _unit_src(self, desc):
        key, sub, off, n = desc[1]
        W = self.I[key] if sub is None else self.I[key][sub]
        if desc[0][0] == "k":
            return W.rearrange("(kc p) n -> p kc n", p=128)[:, :, off:off + n]
        return W.rearrange("(fc p) n -> p fc n", p=128)[:, off:off + n, :]

    def _slot_view(self, s, kind):
        slot = self.WR[s]
        if kind[0] == "k":
            return slot[:, 0:8 * kind[1]].rearrange("p (k n) -> p k n", k=8)
        return slot[:, 0:kind[1] * 1024].rearrange("p (f n) -> p f n", f=kind[1])

    def _issue_unit(self, i):
        kind = self.plan[i][0]
        src = self._unit_src(self.plan[i])
        s = i % NSLOT
        dst = self._slot_view(s, kind)
        self.P.add(POOL, lambda e, dst=dst, src=src: e.dma_start(out=dst, in_=src),
                   writes=[("W%d" % s, 0, 1)], dma="w%d" % s)

    def next_unit(self, kind, desc, live_prev=0):
        i = self.uidx
        self.uidx += 1
        if self.record:
            self.plan.append((kind, desc))
        else:
            assert self.plan[i] == (kind, desc), (i, self.plan[i], kind, desc)
            while self.issued < min(len(self.plan), i + NSLOT - live_prev):
                self._issue_unit(self.issued)
                self.issued += 1
        s = i % NSLOT
        return self._slot_view(s, kind), ("W%d" % s, 0, 1)

    def kunits(self, key, sub, ncols_total, c0=0, step=512):
        out = []
        c = c0
        while c < c0 + ncols_total:
            n = min(step, c0 + ncols_total - c)
            out.append((("k", n), (key, sub, c, n)))
            c += n
        return out

    def mm_group(self, ps, T, pairs, reads, pr):
        n = len(pairs)

        def mm(e):
            ins = None
            for i, (l, r) in enumerate(pairs):
                ins = e.matmul(ps[:, 0:T], lhsT=l, rhs=r, start=(i == 0), stop=(i == n - 1))
            return ins
        self.P.add(PE, mm, reads=reads, writes=[pr])

    def build(self):
        nc = bass.Bass("TRN2", target_bir_lowering=False)
        self.nc = nc
        mode = self.mode
        P = self.P
        dt = nc.dram_tensor
        I = {}
        if mode in ("L0", "FUSED"):
            I["xT"] = dt("xT", [D, N0], F32, kind="ExternalInput").ap()
        else:
            I["h1T"] = dt("h1T", [D, NR1], F32, kind="ExternalInput").ap()
        I["maskb"] = dt("maskb", [128, N0], F32, kind="ExternalInput").ap()
        I["memT"] = dt("memT", [D, MEM], F32, kind="ExternalInput").ap()
        I["vecs"] = dt("vecs", [128, NVEC, 8], F32, kind="ExternalInput").ap()
        I["ident"] = dt("ident", [128, 128], F32, kind="ExternalInput").ap()
        I["xa_wq"] = dt("xa_wq", [2, D, D], F32, kind="ExternalInput").ap()
        I["xa_wkv"] = dt("xa_wkv", [2, D, 2 * D], F32, kind="ExternalInput").ap()
        I["xa_wo"] = dt("xa_wo", [2, D, D], F32, kind="ExternalInput").ap()
        if mode in ("L0", "FUSED"):
            I["w_in"] = dt("w_in", [D, 2048], F32, kind="ExternalInput").ap()
            I["pool_w"] = dt("pool_w", [4, 128, 128], F32, kind="ExternalInput").ap()
            I["w_out"] = dt("w_out", [D, D], F32, kind="ExternalInput").ap()
            I["ffn_gu"] = dt("ffn_gu", [D, 2 * DFF], F32, kind="ExternalInput").ap()
            I["ffn_down"] = dt("ffn_down", [DFF, D], F32, kind="ExternalInput").ap()
        if mode in ("L1", "FUSED"):
            I["pw1"] = dt("pw1", [D, 2048], F32, kind="ExternalInput").ap()
            I["pw2"] = dt("pw2", [D, D], F32, kind="ExternalInput").ap()
            I["router"] = dt("router", [D, NEXP], F32, kind="ExternalInput").ap()
            I["moe_gu"] = dt("moe_gu", [NEXP, D, 2 * DFFE], F32, kind="ExternalInput").ap()
            I["moe_down"] = dt("moe_down", [NEXP, DFFE, D], F32, kind="ExternalInput").ap()
        if mode == "L0":
            OUT = dt("h1o", [D, NR1], F32, kind="ExternalOutput").ap()
        else:
            OUT = dt("outT", [D, OWN], F32, kind="ExternalOutput").ap()
        self.I = I

        with ExitStack() as st:
            sb = lambda name, shape, dtype: st.enter_context(nc.sbuf_tensor(name, shape, dtype))
            self.H = sb("H", [128, NCH, N0], F32)
            self.XN = sb("XN", [128, NCH, N0], BF16)
            self.MASK = sb("MASK", [128, N0], BF16)
            self.WR = [sb("WR%d" % i, [128, 4096], BF16) for i in range(NSLOT)]
            self.A = sb("A", [128, NCH * NR1], BF16)
            self.KT = sb("KT", [128, NCH, MEM], BF16)
            self.V = sb("V", [128, 2, D], BF16)
            self.VEC = sb("VEC", [128, NVEC, 8], F32)
            self.POOLW = sb("POOLW", [128, 4, 128], BF16)
            self.ONES = sb("ONES", [128, 128], BF16)
            self.ONESF = sb("ONESF", [128, 128], F32)
            self.IDENT = sb("IDENT", [128, 128], F32)
            self.EPSV = sb("EPSV", [128, 1], F32)
            self.RT = [sb("RT%d" % i, [128, 512], F32) for i in range(2)]
            self.RS = [sb("RS%d" % i, [128, 512], F32) for i in range(2)]
            self.B = sb("B", [128, BW], F32)
            self.C = sb("C", [128, CW], F32)
            self.PS = [st.enter_context(nc.psum_tensor("ps%d" % i, [128, 512], F32)) for i in range(8)]

            self.prologue()
            if mode in ("L0", "FUSED"):
                self.layer0()
            if mode in ("L1", "FUSED"):
                self.layer1()
            self.epilogue(OUT)
            if not self.record:
                self.stats = P.emit(nc, final_waits=["out"])
        return nc

    def prologue(self):
        P, I = self.P, self.I
        P.add(SP, lambda e: e.dma_start(out=self.VEC[:], in_=I["vecs"]), writes=[("VEC", 0, 1)], dma="c")
        P.add(SP, lambda e: e.dma_start(out=self.IDENT[:], in_=I["ident"]), writes=[("IDENT", 0, 1)], dma="c")
        hN = N0 // 2
        P.add(POOL, lambda e: e.dma_start(out=self.MASK[:, 0:hN], in_=I["maskb"][:, 0:hN]), writes=[("MASK", 0, hN)], dma="c2")
        P.add(POOL, lambda e: e.dma_start(out=self.MASK[:, hN:N0], in_=I["maskb"][:, hN:N0]), writes=[("MASK", hN, N0)], dma="c2")
        P.add(DVE, lambda e: e.memset(self.ONES[:], 1.0), writes=[("ONES", 0, 1)])
        P.add(DVE, lambda e: e.memset(self.ONESF[:], 1.0), writes=[("ONESF", 0, 1)])
        P.add(DVE, lambda e: e.memset(self.EPSV[:], EPS), writes=[("EPSV", 0, 1)])
        if self.mode in ("L0", "FUSED"):
            xv = I["xT"].rearrange("(c p) t -> p c t", p=128)
            for ti, (lo, hi) in enumerate(TN0):
                for c in range(NCH):
                    P.add(SP, lambda e, lo=lo, hi=hi, c=c: e.dma_start(out=self.H[:, c, lo:hi], in_=xv[:, c, lo:hi]),
                          writes=[("H%d" % c, lo, hi)], dma="x%d" % ti)
            P.add(POOL, lambda e: e.dma_start(out=self.POOLW[:], in_=I["pool_w"].rearrange("g c d -> c g d")),
                  writes=[("POOLW", 0, 1)], dma="c2")
        else:
            hv = I["h1T"].rearrange("(c p) t -> p c t", p=128)
            for c in range(NCH):
                P.add(SP, lambda e, c=c: e.dma_start(out=self.H[:, c, R1LO:R1HI], in_=hv[:, c, :]),
                      writes=[("H%d" % c, R1LO, R1HI)], dma="x")

    def epilogue(self, OUT):
        P = self.P
        ov = OUT.rearrange("(c p) t -> p c t", p=128)
        if self.mode == "L0":
            lo, hi = R1LO, R1HI
            for c in range(NCH):
                P.add(SP, lambda e, c=c: e.dma_start(out=ov[:, c, :], in_=self.H[:, c, lo:hi]),
                      reads=[("H%d" % c, lo, hi)], dma="out")
        else:
            for (lo, hi) in T1:
                for c in range(NCH):
                    P.add(SP, lambda e, lo=lo, hi=hi, c=c: e.dma_start(out=ov[:, c, lo - OWN0:hi - OWN0], in_=self.H[:, c, lo:hi]),
                          reads=[("H%d" % c, lo, hi)], dma="out")

    def rmsnorm(self, vidx, tiles, out_f32_inplace=False, save_rstd=False):
        P = self.P
        for (lo, hi) in tiles:
            T = hi - lo
            for c in range(NCH):
                P.add(ACT, lambda e, c=c, lo=lo, hi=hi: e.activation(out=self.XN[:, c, lo:hi], in_=self.H[:, c, lo:hi], func=AF.Square),
                      reads=[("H%d" % c, lo, hi)], writes=[("XN%d" % c, lo, hi)])
            ps, pr = self.bank()
            self.mm_group(ps, T, [(self.ONES[:, :], self.XN[:, c, lo:hi]) for c in range(NCH)],
                          [("XN%d" % c, lo, hi) for c in range(NCH)] + [("ONES", 0, 1)], pr)
            r = self.rotate("rt", 2)
            RT, RS = self.RT[r], self.RS[r]
            P.add(ACT, lambda e, ps=ps, RT=RT, T=T: e.activation(out=RT[:, 0:T], in_=ps[:, 0:T], func=AF.Ln, scale=1.0 / D, bias=self.EPSV[:, 0:1]),
                  reads=[pr, ("EPSV", 0, 1)], writes=[("RT%d" % r, 0, 1)])
            if save_rstd:
                rsap, rsr = self.Bv(lo - OWN0, hi - OWN0)
            else:
                rsap, rsr = RS[:, 0:T], ("RS%d" % r, 0, 1)
            P.add(ACT, lambda e, RT=RT, T=T, rsap=rsap: e.activation(out=rsap, in_=RT[:, 0:T], func=AF.Exp, scale=-0.5), reads=[("RT%d" % r, 0, 1)], writes=[rsr])
            for c in range(NCH):
                if out_f32_inplace:
                    out = self.H[:, c, lo:hi]
                    wr = [("H%d" % c, lo, hi)]
                else:
                    out = self.XN[:, c, lo:hi]
                    wr = [("XN%d" % c, lo, hi)]
                P.add(DVE, lambda e, c=c, lo=lo, hi=hi, out=out, rsap=rsap: e.scalar_tensor_tensor(
                    out=out, in0=self.H[:, c, lo:hi], scalar=self.VEC[:, vidx, c:c + 1], in1=rsap, op0=ALU.mult, op1=ALU.mult),
                    reads=[("H%d" % c, lo, hi), rsr, ("VEC", 0, 1)], writes=wr)

    def linear_residual(self, units, src, srcr, tiles, bias_vidx=None, src_off=0):
        P = self.P
        oc_base = 0
        for (kind, desc) in units:
            wv, wr = self.next_unit(kind, desc)
            nco = kind[1] // 128
            for j in range(nco):
                oc = oc_base + j
                for (lo, hi) in tiles:
                    T = hi - lo
                    ps, pr = self.bank()
                    self.mm_group(ps, T, [(wv[:, k, j * 128:(j + 1) * 128], src[:, k, lo - src_off:hi - src_off]) for k in range(NCH)],
                                  [wr] + [srcr(k, lo, hi) for k in range(NCH)], pr)
                    if bias_vidx is None:
                        P.add(DVE, lambda e, ps=ps, oc=oc, lo=lo, hi=hi, T=T: e.tensor_tensor(
                            out=self.H[:, oc, lo:hi], in0=ps[:, 0:T], in1=self.H[:, oc, lo:hi], op=ALU.add),
                            reads=[pr, ("H%d" % oc, lo, hi)], writes=[("H%d" % oc, lo, hi)])
                    else:
                        P.add(DVE, lambda e, ps=ps, oc=oc, lo=lo, hi=hi, T=T: e.scalar_tensor_tensor(
                            out=self.H[:, oc, lo:hi], in0=ps[:, 0:T], scalar=self.VEC[:, bias_vidx, oc:oc + 1], in1=self.H[:, oc, lo:hi],
                            op0=ALU.add, op1=ALU.add),
                            reads=[pr, ("H%d" % oc, lo, hi), ("VEC", 0, 1)], writes=[("H%d" % oc, lo, hi)])
            oc_base += nco

    def mixer0(self):
        P = self.P
        YC = self.A[:, :].rearrange("p (c t) -> p c t", c=NCH)
        W = 432
        T1s = [self.Bv(i * W, (i + 1) * W) for i in range(2)]
        GBs = [self.Bv((2 + i) * W, (3 + i) * W) for i in range(2)]
        Us = [self.Bv((4 + i) * W, (5 + i) * W) for i in range(2)]
        (Cb, rC), (T2b, rT2), (S1b, rS1), (S2b, rS2) = [self.Bv((6 + i) * W, (7 + i) * W) for i in range(4)]
        (K1b, rK1), (K2b, rK2), (PEb, rPE) = [self.Bv(10 * W + 64 * i, 10 * W + 64 * (i + 1)) for i in range(3)]
        PBs = []
        for i in range(2):
            f_, r_ = self.Cv(256 * i, 256 * (i + 1))
            PBs.append((f_.bitcast(BF16), r_))
        units = self.kunits("w_in", None, 2048)
        VEC = self.VEC
        pending = []
        for i in range(4):
            wv, wr = self.next_unit(*units[i])
            win = 2 << i
            left = win // 2
            for (lo, hi) in T0:
                T = hi - lo
                elo, ehi = lo - 8, hi + 7
                TE = ehi - elo
                banks = [self.bank() for _ in range(4)]
                for j in range(4):
                    ps, pr = banks[j]
                    self.mm_group(ps, TE, [(wv[:, k, j * 128:(j + 1) * 128], self.XN[:, k, elo:ehi]) for k in range(NCH)],
                                  [wr] + [("XN%d" % k, elo, ehi) for k in range(NCH)], pr)
                (ph, rh), (pgb, rgb), (pgc, rgc), (pu, ru) = banks
                while len(pending) > 0:
                    pending.pop(0)()
                bi_ = self.rotate("mx", 2)
                (T1b, rT1), (GBb, rGB), (Ub, rU) = T1s[bi_], GBs[bi_], Us[bi_]
                PBb, rPB = PBs[bi_]
                P.add(ACT, lambda e, ph=ph, TE=TE, T1b=T1b: e.copy(out=T1b[:, 0:TE], in_=ph[:, 0:TE]), reads=[rh], writes=[rT1])
                P.add(DVE, lambda e, pgc=pgc, TE=TE, T1b=T1b: e.tensor_tensor(out=Cb[:, 0:TE], in0=pgc[:, 0:TE], in1=T1b[:, 0:TE], op=ALU.mult),
                      reads=[rgc, rT1], writes=[rC])
                P.add(ACT, lambda e, pgb=pgb, T=T, GBb=GBb: e.copy(out=GBb[:, 0:T], in_=pgb[:, 8:8 + T]), reads=[rgb], writes=[rGB])
                P.add(ACT, lambda e, pu=pu, TE=TE, Ub=Ub: e.copy(out=Ub[:, 0:TE], in_=pu[:, 0:TE]), reads=[ru], writes=[rU])
                P.add(DVE, lambda e, T=T, i=i: e.tensor_scalar(out=T2b[:, 0:T], in0=Cb[:, 7:7 + T], scalar1=VEC[:, V_CONVA + 0, i:i + 1], scalar2=None, op0=ALU.mult),
                      reads=[rC, ("VEC", 0, 1)], writes=[rT2])
                for kk in (1, 2):
                    P.add(DVE, lambda e, T=T, i=i, kk=kk: e.scalar_tensor_tensor(
                        out=T2b[:, 0:T], in0=Cb[:, 7 + kk:7 + kk + T], scalar=VEC[:, V_CONVA + kk, i:i + 1], in1=T2b[:, 0:T],
                        op0=ALU.mult, op1=ALU.add), reads=[rC, rT2, ("VEC", 0, 1)], writes=[rT2])
                P.add(DVE, lambda e, T=T, i=i, lo=lo, hi=hi, GBb=GBb: e.tensor_tensor(out=YC[:, i, lo - R1LO:hi - R1LO], in0=T2b[:, 0:T], in1=GBb[:, 0:T], op=ALU.mult),
                      reads=[rT2, rGB], writes=[rYC(i, lo, hi)])
                srcU, rsU = Ub, rU
                n = TE
                step = 1
                bufs = [(S1b, rS1), (S2b, rS2)]
                bi = 0
                while step < win:
                    n2 = n - step
                    dS, rdS = bufs[bi]
                    P.add(DVE, lambda e, srcU=srcU, dS=dS, n2=n2, step=step: e.tensor_tensor(out=dS[:, 0:n2], in0=srcU[:, 0:n2], in1=srcU[:, step:step + n2], op=ALU.add),
                          reads=[rsU], writes=[rdS])
                    srcU, rsU = dS, rdS
                    n = n2
                    step *= 2
                    bi ^= 1
                off = 8 - left
                P.add(DVE, lambda e, srcU=srcU, off=off, T=T, win=win, Ub=Ub, PBb=PBb: e.scalar_tensor_tensor(out=PBb[:, 0:T], in0=srcU[:, off:off + T], scalar=1.0 / win, in1=Ub[:, 8:8 + T],
                                                                                             op0=ALU.mult, op1=ALU.subtract),
                      reads=[rsU, rU], writes=[rPB])
                for (a, b) in ((R1LO, R1LO + 32), (R1HI - 32, R1HI)):
                    if not (lo <= a and b <= hi):
                        continue
                    ma, mb = a - 8, b + 7
                    srcK, rsK = None, None
                    nk = mb - ma
                    stepk = 1
                    kb = [(K1b, rK1), (K2b, rK2)]
                    ki = 0
                    while stepk < win:
                        n2 = nk - stepk
                        dK, rdK = kb[ki]
                        if srcK is None:
                            P.add(DVE, lambda e, dK=dK, n2=n2, stepk=stepk, ma=ma: e.tensor_tensor(out=dK[:, 0:n2], in0=self.MASK[:, ma:ma + n2], in1=self.MASK[:, ma + stepk:ma + stepk + n2], op=ALU.add),
                                  reads=[("MASK", ma, mb)], writes=[rdK])
                        else:
                            P.add(DVE, lambda e, srcK=srcK, dK=dK, n2=n2, stepk=stepk: e.tensor_tensor(out=dK[:, 0:n2], in0=srcK[:, 0:n2], in1=srcK[:, stepk:stepk + n2], op=ALU.add),
                                  reads=[rsK], writes=[rdK])
                        srcK, rsK = dK, rdK
                        nk = n2
                        stepk *= 2
                        ki ^= 1
                    P.add(DVE, lambda e, srcK=srcK, off=off: e.tensor_scalar(out=PEb[:, 0:32], in0=srcK[:, off:off + 32], scalar1=1.0, scalar2=None, op0=ALU.max),
                          reads=[rsK], writes=[rPE])
                    P.add(DVE, lambda e: e.reciprocal(out=PEb[:, 0:32], in_=PEb[:, 0:32]), reads=[rPE], writes=[rPE])
                    eo = a - elo
                    P.add(DVE, lambda e, srcU=srcU, eo=eo, left=left: e.tensor_tensor(out=PEb[:, 0:32], in0=srcU[:, eo - left:eo - left + 32], in1=PEb[:, 0:32], op=ALU.mult),
                          reads=[rsU, rPE], writes=[rPE])
                    P.add(DVE, lambda e, eo=eo, a=a, lo=lo, Ub=Ub, PBb=PBb: e.tensor_tensor(out=PBb[:, a - lo:a - lo + 32], in0=PEb[:, 0:32], in1=Ub[:, eo:eo + 32], op=ALU.subtract),
                          reads=[rPE, rU, rPB], writes=[rPB])
                def pool_tail(T=T, i=i, lo=lo, hi=hi, PBb=PBb, rPB=rPB):
                    ps, pr = self.bank()
                    P.add(PE, lambda e, ps=ps: e.matmul(ps[:, 0:T], lhsT=self.POOLW[:, i, :], rhs=PBb[:, 0:T], start=True, stop=True),
                          reads=[rPB, ("POOLW", 0, 1)], writes=[pr])
                    P.add(ACT, lambda e, ps=ps: e.activation(out=YC[:, 4 + i, lo - R1LO:hi - R1LO], in_=ps[:, 0:T], func=AF.Identity,
                                                             scale=VEC[:, V_POOL_SCALE, i:i + 1]),
                          reads=[pr, ("VEC", 0, 1)], writes=[rYC(4 + i, lo, hi)])
                pending.append(pool_tail)
        while pending:
            pending.pop(0)()
        self.linear_residual(self.kunits("w_out", None, D), YC, rYC, T0, src_off=R1LO)

    def xattn(self, L, tiles, vnorm, vnormmem):
        P, I = self.P, self.I
        QT = self.A[:, :].rearrange("p (c t) -> p c t", c=NCH)
        MEMFf, rMEMF = self.Bv(0, 2048)
        MEMF = MEMFf.rearrange("p (c m) -> p c m", c=NCH)
        MEMNf, _ = self.Bv(2048, 3072)
        MEMN = MEMNf.bitcast(BF16).rearrange("p (c m) -> p c m", c=NCH)
        rMEMN = lambda c0, c1: ("B", 2048 + c0 * 128, 2048 + c1 * 128)
        ESf, _ = self.Bv(3072, 4096)
        ESb = ESf.bitcast(BF16)
        rES = lambda i: ("B", 3072 + i * 256, 3072 + (i + 1) * 256)
        P.add(SP, lambda e: e.dma_start(out=MEMF, in_=I["memT"].rearrange("(c p) m -> p c m", p=128)), writes=[rMEMF], dma="m%d" % L)
        for c in range(NCH):
            P.add(ACT, lambda e, c=c: e.activation(out=MEMN[:, c, :], in_=MEMF[:, c, :], func=AF.Square), reads=[rMEMF], writes=[rMEMN(c, c + 1)])
        ps, pr = self.bank()
        self.mm_group(ps, MEM, [(self.ONES[:, :], MEMN[:, c, :]) for c in range(NCH)], [rMEMN(0, NCH), ("ONES", 0, 1)], pr)
        r = self.rotate("rt", 2)
        RT, RS = self.RT[r], self.RS[r]
        P.add(ACT, lambda e, ps=ps, RT=RT: e.activation(out=RT[:, 0:MEM], in_=ps[:, 0:MEM], func=AF.Ln, scale=1.0 / D, bias=self.EPSV[:, 0:1]),
              reads=[pr, ("EPSV", 0, 1)], writes=[("RT%d" % r, 0, 1)])
        P.add(ACT, lambda e, RT=RT, RS=RS: e.activation(out=RS[:, 0:MEM], in_=RT[:, 0:MEM], func=AF.Exp, scale=-0.5), reads=[("RT%d" % r, 0, 1)], writes=[("RS%d" % r, 0, 1)])
        for c in range(NCH):
            P.add(DVE, lambda e, c=c, RS=RS: e.scalar_tensor_tensor(out=MEMN[:, c, :], in0=MEMF[:, c, :], scalar=self.VEC[:, vnormmem, c:c + 1], in1=RS[:, 0:MEM],
                                                                    op0=ALU.mult, op1=ALU.mult),
                  reads=[rMEMF, ("RS%d" % r, 0, 1), ("VEC", 0, 1)], writes=[rMEMN(c, c + 1)])
        kvunits = self.kunits("xa_wkv", L, 2 * D)
        for u in range(2):
            wv, wr = self.next_unit(*kvunits[u])
            for j in range(4):
                oc = u * 4 + j
                ps, pr = self.bank()
                self.mm_group(ps, MEM, [(wv[:, k, j * 128:(j + 1) * 128], MEMN[:, k, :]) for k in range(NCH)], [wr, rMEMN(0, NCH)], pr)
                P.add(ACT, lambda e, ps=ps, oc=oc: e.copy(out=self.KT[:, oc, :], in_=ps[:, 0:MEM]), reads=[pr], writes=[("KT", oc, oc + 1)])
        for u in range(2):
            wv, wr = self.next_unit(*kvunits[2 + u])
            for mc in range(2):
                ps, pr = self.bank()
                self.mm_group(ps, 512, [(MEMN[:, k, mc * 128:(mc + 1) * 128], wv[:, k, :]) for k in range(NCH)], [wr, rMEMN(0, NCH)], pr)
                P.add(DVE, lambda e, ps=ps, mc=mc, u=u: e.tensor_copy(out=self.V[:, mc, u * 512:(u + 1) * 512], in_=ps[:, 0:512]),
                      reads=[pr], writes=[("V", mc * 2 + u, mc * 2 + u + 1)])
        self.rmsnorm(vnorm, tiles)
        qunits = self.kunits("xa_wq", L, D)
        ev = 0
        for u in range(2):
            wv, wr = self.next_unit(*qunits[u])
            for j in range(4):
                oc = u * 4 + j
                for (lo, hi) in tiles:
                    T = hi - lo
                    ps, pr = self.bank()
                    self.mm_group(ps, T, [(wv[:, k, j * 128:(j + 1) * 128], self.XN[:, k, lo:hi]) for k in range(NCH)],
                                  [wr] + [("XN%d" % k, lo, hi) for k in range(NCH)], pr)
                    if ev % 2 == 0:
                        P.add(ACT, lambda e, ps=ps, oc=oc, lo=lo, hi=hi, T=T: e.copy(out=QT[:, oc, lo - R1LO:hi - R1LO], in_=ps[:, 0:T]),
                              reads=[pr], writes=[rYC(oc, lo, hi)])
                    else:
                        P.add(DVE, lambda e, ps=ps, oc=oc, lo=lo, hi=hi, T=T: e.tensor_copy(out=QT[:, oc, lo - R1LO:hi - R1LO], in_=ps[:, 0:T]),
                              reads=[pr], writes=[rYC(oc, lo, hi)])
                    ev += 1
        apend = []
        for (lo, hi) in tiles:
            T = hi - lo
            for h in range(4):
                es = self.rotate("es", 2)
                ES = [ESb[:, (es * 2 + mc) * 512:(es * 2 + mc) * 512 + 512] for mc in range(2)]
                rESl = [rES(es * 2 + mc) for mc in range(2)]
                RSM, rRSM = self.Bv(4096 + es * 512, 4096 + es * 512 + 512)
                for mc in range(2):
                    ps, pr = self.bank()
                    self.mm_group(ps, T, [(self.KT[:, 2 * h + hc, mc * 128:(mc + 1) * 128], QT[:, 2 * h + hc, lo - R1LO:hi - R1LO]) for hc in range(2)],
                                  [("KT", 2 * h, 2 * h + 2), rYC(2 * h, lo, hi), rYC(2 * h + 1, lo, hi)], pr)
                    P.add(ACT, lambda e, ps=ps, mc=mc, T=T, ES=ES: e.activation(out=ES[mc][:, 0:T], in_=ps[:, 0:T], func=AF.Exp, scale=1.0 / 16.0),
                          reads=[pr], writes=[rESl[mc]])
                while apend:
                    apend.pop(0)()

                def attn_tail(T=T, ES=ES, rESl=rESl, RSM=RSM, rRSM=rRSM, h=h, lo=lo, hi=hi):
                    ps, pr = self.bank()
                    self.mm_group(ps, T, [(self.ONES[:, :], ES[mc][:, 0:T]) for mc in range(2)], [rESl[0], rESl[1], ("ONES", 0, 1)], pr)
                    P.add(ACT, lambda e, ps=ps: e.activation(out=RSM[:, 0:T], in_=ps[:, 0:T], func=AF.Ln), reads=[pr], writes=[rRSM])
                    P.add(ACT, lambda e: e.activation(out=RSM[:, 0:T], in_=RSM[:, 0:T], func=AF.Exp, scale=-1.0), reads=[rRSM], writes=[rRSM])
                    for hc in range(2):
                        oc = 2 * h + hc
                        ps, pr = self.bank()
                        self.mm_group(ps, T, [(self.V[:, mc, oc * 128:(oc + 1) * 128], ES[mc][:, 0:T]) for mc in range(2)],
                                      [("V", 0, 4), rESl[0], rESl[1]], pr)
                        P.add(DVE, lambda e, ps=ps, oc=oc: e.tensor_tensor(out=QT[:, oc, lo - R1LO:hi - R1LO], in0=ps[:, 0:T], in1=RSM[:, 0:T], op=ALU.mult),
                              reads=[pr, rRSM], writes=[rYC(oc, lo, hi)])
                apend.append(attn_tail)
        while apend:
            apend.pop(0)()
        self.linear_residual(self.kunits("xa_wo", L, D), QT, rYC, tiles, src_off=R1LO)

    def swiglu(self, key, sub, dff, tiles, crow=None, crowr=None, mid_hook=None):
        P = self.P
        nfc = dff // 128
        HT = [self.A[:, b * 4 * NR1:(b + 1) * 4 * NR1].rearrange("p (j t) -> p j t", j=4) for b in range(2)]
        SGf, _ = self.Cv(0, 512)
        SGb = SGf.bitcast(BF16)
        rSG = lambda s: ("C", s * 256, (s + 1) * 256)
        t0 = tiles[0][0]
        g0 = 0
        while g0 < nfc:
            ng = min(4, nfc - g0)
            hb = self.rotate("ht", 2)
            wg, wgr = self.next_unit(("k", ng * 128), (key, sub, g0 * 128, ng * 128))
            wu, wur = self.next_unit(("k", ng * 128), (key, sub, dff + g0 * 128, ng * 128), live_prev=1)
            for j in range(ng):
                for (lo, hi) in tiles:
                    T = hi - lo
                    pg, pgr = self.bank()
                    pu, pur = self.bank()
                    for (ps, pr, wv, wr) in ((pg, pgr, wg, wgr), (pu, pur, wu, wur)):
                        self.mm_group(ps, T, [(wv[:, k, j * 128:(j + 1) * 128], self.XN[:, k, lo:hi]) for k in range(NCH)],
                                      [wr] + [("XN%d" % k, lo, hi) for k in range(NCH)], pr)
                    s = self.rotate("sg", 2)
                    SG = SGb[:, s * 512:s * 512 + 512]
                    P.add(ACT, lambda e, pg=pg, SG=SG, T=T: e.activation(out=SG[:, 0:T], in_=pg[:, 0:T], func=AF.Silu), reads=[pgr], writes=[rSG(s)])
                    hdst = HT[hb][:, j, lo - t0:hi - t0]
                    hr = rHT(hb, j, lo, hi, t0)
                    if crow is None:
                        P.add(DVE, lambda e, pu=pu, SG=SG, T=T, hdst=hdst: e.tensor_tensor(out=hdst, in0=pu[:, 0:T], in1=SG[:, 0:T], op=ALU.mult),
                              reads=[pur, rSG(s)], writes=[hr])
                    else:
                        TU, rTU = self.Cv(512 + s * 512, 1024 + s * 512)
                        P.add(DVE, lambda e, pu=pu, TU=TU, T=T, lo=lo, hi=hi: e.tensor_tensor(out=TU[:, 0:T], in0=pu[:, 0:T], in1=crow[:, lo - OWN0:hi - OWN0], op=ALU.mult),
                              reads=[pur, crowr(lo, hi)], writes=[rTU])
                        P.add(DVE, lambda e, SG=SG, TU=TU, T=T, hdst=hdst: e.tensor_tensor(out=hdst, in0=TU[:, 0:T], in1=SG[:, 0:T], op=ALU.mult),
                              reads=[rTU, rSG(s)], writes=[hr])
            if mid_hook is not None and g0 == 12:
                mid_hook()
            dkey = {"ffn_gu": "ffn_down", "moe_gu": "moe_down"}[key]
            wd, wdr = self.next_unit(("d", ng), (dkey, sub, g0, ng))
            for oc in range(NCH):
                for (lo, hi) in tiles:
                    T = hi - lo
                    ps, pr = self.bank()
                    self.mm_group(ps, T, [(wd[:, j, oc * 128:(oc + 1) * 128], HT[hb][:, j, lo - t0:hi - t0]) for j in range(ng)],
                                  [wdr] + [rHT(hb, j, lo, hi, t0) for j in range(ng)], pr)
                    P.add(DVE, lambda e, ps=ps, oc=oc, lo=lo, hi=hi, T=T: e.tensor_tensor(out=self.H[:, oc, lo:hi], in0=ps[:, 0:T], in1=self.H[:, oc, lo:hi], op=ALU.add),
                          reads=[pr, ("H%d" % oc, lo, hi)], writes=[("H%d" % oc, lo, hi)])
            g0 += ng

    def conformer(self):
        P = self.P
        VEC = self.VEC
        CV = self.A[:, 0:NCH * OWN].rearrange("p (c t) -> p c t", c=NCH)
        W = 448
        SIGs = [self.Cv(W * i, W * (i + 1)) for i in range(2)]
        HGfs = [self.Cv(W * (2 + i), W * (3 + i)) for i in range(2)]
        HGbf, _ = self.Cv(W * 4, W * 5)
        HGbb = HGbf.bitcast(BF16)
        rHGb = lambda i: ("C", W * 4 + i * 224, W * 4 + (i + 1) * 224)
        DGW = 31 * 64
        DGs = []
        for i in range(2):
            f, r = self.Bv(i * DGW, (i + 1) * DGW)
            DGs.append((f.bitcast(BF16).rearrange("p (k n) -> p k n", k=31), r))
        TN = [self.Bv(1024 + 512 * i, 1536 + 512 * i) for i in range(2)]
        self.rmsnorm(V_OD_NORM_MIX, T0)
        units = self.kunits("pw1", None, 2048)
        pending = []
        for u in range(4):
            wv, wr = self.next_unit(*units[u])
            for q in range(2):
                c = 2 * u + q
                dgi = self.rotate("dg31", 2)
                DG, rDG = DGs[dgi]
                for kk in range(31):
                    P.add(DVE, lambda e, kk=kk, c=c, DG=DG: e.tensor_scalar(out=DG[:, kk, :], in0=self.IDENT[:, :], scalar1=VEC[:, V_DW + kk, c:c + 1], scalar2=None, op0=ALU.mult),
                          reads=[("IDENT", 0, 1), ("VEC", 0, 1)], writes=[(rDG[0], rDG[1] + kk * 64, rDG[1] + (kk + 1) * 64)])
                for (lo, hi) in TC:
                    T = hi - lo
                    elo, ehi = lo - 15, hi + 15
                    TE = ehi - elo
                    pa, par = self.bank()
                    pg, pgr = self.bank()
                    for (ps, pr, col) in ((pa, par, q), (pg, pgr, 2 + q)):
                        self.mm_group(ps, TE, [(wv[:, k, col * 128:(col + 1) * 128], self.XN[:, k, elo:ehi]) for k in range(NCH)],
                                      [wr] + [("XN%d" % k, elo, ehi) for k in range(NCH)], pr)
                    while pending:
                        pending.pop(0)()
                    si = self.rotate("hg", 2)
                    (SIG, rSIG), (HGf, rHGf) = SIGs[si], HGfs[si]
                    HGb = HGbb[:, si * 448:(si + 1) * 448]
                    P.add(ACT, lambda e, pg=pg, c=c, TE=TE, SIG=SIG: e.activation(out=SIG[:, 0:TE], in_=pg[:, 0:TE], func=AF.Sigmoid, bias=VEC[:, V_PW1_BG, c:c + 1]),
                          reads=[pgr, ("VEC", 0, 1)], writes=[rSIG])
                    P.add(DVE, lambda e, pa=pa, c=c, TE=TE, HGf=HGf, SIG=SIG: e.scalar_tensor_tensor(out=HGf[:, 0:TE], in0=pa[:, 0:TE], scalar=VEC[:, V_PW1_BA, c:c + 1], in1=SIG[:, 0:TE],
                                                                                            op0=ALU.add, op1=ALU.mult),
                          reads=[par, rSIG, ("VEC", 0, 1)], writes=[rHGf])
                    P.add(DVE, lambda e, TE=TE, HGf=HGf, HGb=HGb, elo=elo, ehi=ehi: e.tensor_tensor(out=HGb[:, 0:TE], in0=HGf[:, 0:TE], in1=self.MASK[:, elo:ehi], op=ALU.mult),
                          reads=[rHGf, ("MASK", elo, ehi)], writes=[rHGb(si)])
                    def conv_tail(T=T, DG=DG, rDG=rDG, HGb=HGb, si=si, c=c, lo=lo, hi=hi):
                        ps, pr = self.bank()
                        self.mm_group(ps, T, [(DG[:, kk, :], HGb[:, kk:kk + T]) for kk in range(31)], [rDG, rHGb(si)], pr)
                        P.add(ACT, lambda e, ps=ps: e.activation(out=CV[:, c, lo - OWN0:hi - OWN0], in_=ps[:, 0:T], func=AF.Identity, bias=VEC[:, V_DW_B, c:c + 1]),
                              reads=[pr, ("VEC", 0, 1)], writes=[rCV(c, lo, hi)])
                    pending.append(conv_tail)
        while pending:
            pending.pop(0)()
        pw2u = self.kunits("pw2", None, D)
        pw2w = [self.next_unit(*pw2u[0]), self.next_unit(*pw2u[1], live_prev=1)]
        MUs = [self.Bv(0, 512), self.Bv(2048, 2560)]
        VARs = [self.Bv(512, 1024), self.Bv(2560, 3072)]
        st = {}

        def stage_a1(ti):
            lo, hi = TC[ti]
            T = hi - lo
            for c in range(NCH):
                P.add(ACT, lambda e, c=c: e.activation(out=self.XN[:, c, lo:hi], in_=CV[:, c, lo - OWN0:hi - OWN0], func=AF.Square),
                      reads=[rCV(c, lo, hi)], writes=[("XN%d" % c, lo, hi)])
            p1, p1r = self.bank()
            p2, p2r = self.bank()
            self.mm_group(p1, T, [(self.ONES[:, :], CV[:, c, lo - OWN0:hi - OWN0]) for c in range(NCH)], [rCV(c, lo, hi) for c in range(NCH)] + [("ONES", 0, 1)], p1r)
            self.mm_group(p2, T, [(self.ONES[:, :], self.XN[:, c, lo:hi]) for c in range(NCH)], [("XN%d" % c, lo, hi) for c in range(NCH)] + [("ONES", 0, 1)], p2r)
            r = self.rotate("rt", 2)
            st[ti] = (p1, p1r, p2, p2r, r)

        def stage_a2(ti):
            lo, hi = TC[ti]
            T = hi - lo
            p1, p1r, p2, p2r, r = st[ti]
            (MU, rMU), (VAR, rVAR) = MUs[ti % 2], VARs[ti % 2]
            RT, RS = self.RT[r], self.RS[r]
            P.add(ACT, lambda e: e.activation(out=MU[:, 0:T], in_=p1[:, 0:T], func=AF.Identity, scale=1.0 / D), reads=[p1r], writes=[rMU])
            P.add(DVE, lambda e: e.tensor_tensor(out=VAR[:, 0:T], in0=MU[:, 0:T], in1=MU[:, 0:T], op=ALU.mult), reads=[rMU], writes=[rVAR])
            P.add(DVE, lambda e: e.scalar_tensor_tensor(out=VAR[:, 0:T], in0=p2[:, 0:T], scalar=1.0 / D, in1=VAR[:, 0:T], op0=ALU.mult, op1=ALU.subtract),
                  reads=[p2r, rVAR], writes=[rVAR])
            P.add(DVE, lambda e: e.tensor_scalar(out=VAR[:, 0:T], in0=VAR[:, 0:T], scalar1=0.0, scalar2=None, op0=ALU.max),
                  reads=[rVAR], writes=[rVAR])
            P.add(ACT, lambda e: e.activation(out=RT[:, 0:T], in_=VAR[:, 0:T], func=AF.Ln, bias=self.EPSV[:, 0:1]),
                  reads=[rVAR, ("EPSV", 0, 1)], writes=[("RT%d" % r, 0, 1)])
            P.add(ACT, lambda e: e.activation(out=RS[:, 0:T], in_=RT[:, 0:T], func=AF.Exp, scale=-0.5), reads=[("RT%d" % r, 0, 1)], writes=[("RS%d" % r, 0, 1)])

        def stage_b(ti):
            lo, hi = TC[ti]
            T = hi - lo
            r = st[ti][4]
            (MU, rMU) = MUs[ti % 2]
            RS = self.RS[r]
            for c in range(NCH):
                tn = self.rotate("tn", 2)
                TNb, rTN = TN[tn]
                P.add(DVE, lambda e, c=c, TNb=TNb: e.tensor_tensor(out=TNb[:, 0:T], in0=CV[:, c, lo - OWN0:hi - OWN0], in1=MU[:, 0:T], op=ALU.subtract),
                      reads=[rCV(c, lo, hi), rMU], writes=[rTN])
                P.add(DVE, lambda e, TNb=TNb: e.tensor_tensor(out=TNb[:, 0:T], in0=TNb[:, 0:T], in1=RS[:, 0:T], op=ALU.mult),
                      reads=[rTN, ("RS%d" % r, 0, 1)], writes=[rTN])
                P.add(ACT, lambda e, c=c, TNb=TNb: e.activation(out=self.XN[:, c, lo:hi], in_=TNb[:, 0:T], func=AF.Silu,
                                                                scale=VEC[:, V_LN_G, c:c + 1], bias=VEC[:, V_LN_B, c:c + 1]),
                      reads=[rTN, ("VEC", 0, 1)], writes=[("XN%d" % c, lo, hi)])

        def stage_c(ti):
            lo, hi = TC[ti]
            T = hi - lo
            for u2 in range(2):
                wv2, wr2 = pw2w[u2]
                for j in range(4):
                    oc = u2 * 4 + j
                    ps, pr = self.bank()
                    self.mm_group(ps, T, [(wv2[:, k, j * 128:(j + 1) * 128], self.XN[:, k, lo:hi]) for k in range(NCH)],
                                  [wr2] + [("XN%d" % k, lo, hi) for k in range(NCH)], pr)
                    P.add(DVE, lambda e, ps=ps, oc=oc: e.scalar_tensor_tensor(
                        out=self.H[:, oc, lo:hi], in0=ps[:, 0:T], scalar=VEC[:, V_PW2_B, oc:oc + 1], in1=self.H[:, oc, lo:hi],
                        op0=ALU.add, op1=ALU.add),
                        reads=[pr, ("H%d" % oc, lo, hi), ("VEC", 0, 1)], writes=[("H%d" % oc, lo, hi)])

        nt = len(TC)
        stage_a1(0)
        stage_a2(0)
        for ti in range(nt):
            if ti + 1 < nt:
                stage_a1(ti + 1)
            stage_b(ti)
            if ti + 1 < nt:
                stage_a2(ti + 1)
            stage_c(ti)

    def moe(self):
        P, I = self.P, self.I
        VEC = self.VEC
        RSTDROW, _ = self.Bv(0, OWN)
        CROW, _ = self.Bv(OWN, 2 * OWN)
        rRSTD = lambda lo, hi: ("B", lo - OWN0, hi - OWN0)
        rCROW = lambda lo, hi: ("B", OWN + lo - OWN0, OWN + hi - OWN0)
        GRf, rGR = self.Cv(1536, 1600)
        GR = GRf.rearrange("p (c e) -> p c e", c=NCH)
        RTRf, rRTR = self.Cv(1600, 1664)
        RTR = RTRf.rearrange("p (c e) -> p c e", c=NCH)
        CBf, _ = self.Cv(1664, 1792)
        CBALL = CBf.rearrange("p (t e) -> p t e", t=16)
        rCB = lambda tt: ("C", 1664 + tt * 8, 1672 + tt * 8)
        (Lb, rL), (EQ1, rEQ1), (L2, rL2), (EQ2, rEQ2) = [self.Cv(1792 + 8 * i, 1800 + 8 * i) for i in range(4)]
        (M1, rM1), (M2, rM2), (DM, rDM), (EE, rEE), (P1, rP1), (P2, rP2), (RTOK, rRTOK) = [self.Cv(1824 + i, 1825 + i) for i in range(7)]
        DIAG = [self.Cv(1840 + 128 * i, 1968 + 128 * i) for i in range(2)]
        self.rmsnorm(V_NORM_MOE, T1, save_rstd=True)
        P.add(SP, lambda e: e.dma_start(out=RTR, in_=I["router"].rearrange("(c p) e -> p c e", p=128)), writes=[rRTR], dma="rt")
        for c in range(NCH):
            P.add(DVE, lambda e, c=c: e.tensor_scalar(out=GR[:, c, :], in0=RTR[:, c, :], scalar1=VEC[:, V_NORM_MOE, c:c + 1], scalar2=None, op0=ALU.mult),
                  reads=[rRTR, ("VEC", 0, 1)], writes=[("C", 1536 + 8 * c, 1544 + 8 * c)])
        for tt in range(16):
            lo = OWN0 + 128 * tt
            hi = lo + 128
            ps, pr = self.bank()
            self.mm_group(ps, NEXP, [(self.H[:, c, lo:hi], GR[:, c, :]) for c in range(NCH)], [("H%d" % c, lo, hi) for c in range(NCH)] + [rGR], pr)
            ps2, pr2 = self.bank()
            P.add(PE, lambda e, ps2=ps2, lo=lo, hi=hi: e.matmul(ps2[:, 0:1], lhsT=RSTDROW[0:1, lo - OWN0:hi - OWN0], rhs=self.ONESF[0:1, 0:1], start=True, stop=True),
                  reads=[rRSTD(lo, hi), ("ONESF", 0, 1)], writes=[pr2])
            P.add(ACT, lambda e, ps=ps: e.copy(out=Lb, in_=ps[:, 0:NEXP]), reads=[pr], writes=[rL])
            P.add(ACT, lambda e, ps2=ps2: e.copy(out=RTOK, in_=ps2[:, 0:1]), reads=[pr2], writes=[rRTOK])
            P.add(DVE, lambda e: e.reduce_max(out=M1, in_=Lb, axis=AX.X), reads=[rL], writes=[rM1])
            P.add(DVE, lambda e: e.tensor_scalar(out=EQ1, in0=Lb, scalar1=M1, scalar2=None, op0=ALU.is_equal), reads=[rL, rM1], writes=[rEQ1])
            P.add(DVE, lambda e: e.scalar_tensor_tensor(out=L2, in0=EQ1, scalar=-1.0e30, in1=Lb, op0=ALU.mult, op1=ALU.add), reads=[rEQ1, rL], writes=[rL2])
            P.add(DVE, lambda e: e.reduce_max(out=M2, in_=L2, axis=AX.X), reads=[rL2], writes=[rM2])
            P.add(DVE, lambda e: e.tensor_scalar(out=EQ2, in0=L2, scalar1=M2, scalar2=None, op0=ALU.is_equal), reads=[rL2, rM2], writes=[rEQ2])
            P.add(DVE, lambda e: e.tensor_tensor(out=DM, in0=M2, in1=M1, op=ALU.subtract), reads=[rM1, rM2], writes=[rDM])
            P.add(ACT, lambda e: e.activation(out=EE, in_=DM, func=AF.Exp, scale=RTOK), reads=[rDM, rRTOK], writes=[rEE])
            P.add(DVE, lambda e: e.tensor_scalar(out=P1, in0=EE, scalar1=1.0, scalar2=None, op0=ALU.add), reads=[rEE], writes=[rP1])
            P.add(DVE, lambda e: e.reciprocal(out=P1, in_=P1), reads=[rP1], writes=[rP1])
            P.add(DVE, lambda e: e.tensor_tensor(out=P2, in0=EE, in1=P1, op=ALU.mult), reads=[rEE, rP1], writes=[rP2])
            P.add(DVE, lambda e, tt=tt: e.tensor_scalar(out=CBALL[:, tt, :], in0=EQ1, scalar1=P1, scalar2=None, op0=ALU.mult),
                  reads=[rEQ1, rP1], writes=[rCB(tt)])
            P.add(DVE, lambda e, tt=tt: e.scalar_tensor_tensor(out=CBALL[:, tt, :], in0=EQ2, scalar=P2, in1=CBALL[:, tt, :], op0=ALU.mult, op1=ALU.add),
                  reads=[rEQ2, rP2, rCB(tt)], writes=[rCB(tt)])
        CROWS = [(CROW, rCROW), (RSTDROW, rRSTD)]

        def make_crow(ex):
            crow, crowr = CROWS[ex % 2]
            for tt in range(16):
                lo = OWN0 + 128 * tt
                hi = lo + 128
                dg = self.rotate("dg", 2)
                DG, rDG = DIAG[dg]
                P.add(DVE, lambda e, tt=tt, DG=DG: e.tensor_scalar(out=DG, in0=self.IDENT[:, :], scalar1=CBALL[:, tt, ex:ex + 1], scalar2=None, op0=ALU.mult),
                      reads=[("IDENT", 0, 1), rCB(tt)], writes=[rDG])
                ps, pr = self.bank()
                P.add(PE, lambda e, ps=ps, DG=DG: e.matmul(ps[:, 0:128], lhsT=self.ONESF[:, :], rhs=DG, start=True, stop=True),
                      reads=[rDG, ("ONESF", 0, 1)], writes=[pr])
                P.add(ACT, lambda e, ps=ps, lo=lo, hi=hi: e.copy(out=crow[:, lo - OWN0:hi - OWN0], in_=ps[:, 0:128]), reads=[pr], writes=[crowr(lo, hi)])

        make_crow(0)
        for ex in range(NEXP):
            crow, crowr = CROWS[ex % 2]
            hook = (lambda ex=ex: make_crow(ex + 1)) if ex + 1 < NEXP else None
            self.swiglu("moe_gu", ex, DFFE, T1, crow=crow, crowr=crowr, mid_hook=hook)

    def layer0(self):
        self.rmsnorm(V_EV_NORM_MIX, TN0)
        self.mixer0()
        if DEBUG_STOP == "mix":
            return
        self.xattn(0, T0, V_XA_NORM0, V_XA_NORMMEM0)
        if DEBUG_STOP == "xa0":
            return
        self.rmsnorm(V_EV_NORM_FFN, T0)
        self.swiglu("ffn_gu", None, DFF, T0)

    def layer1(self):
        self.conformer()
        if DEBUG_STOP == "conf":
            return
        self.xattn(1, T1, V_XA_NORM1, V_XA_NORMMEM1)
        if DEBUG_STOP == "xa1":
            return
        self.moe()
        self.rmsnorm(V_FINAL, T1, out_f32_inplace=True)


def build_program(mode):
    b1 = Builder(mode)
    b1.build()
    b2 = Builder(mode, unit_plan=b1.plan)
    nc = b2.build()
    return nc, b2


def _feat(v):
    return np.ascontiguousarray(np.asarray(v, np.float32).reshape(8, 128).T)


def _feat_small(v, n):
    out = np.zeros((128, 8), np.float32)
    out[:, :n] = np.asarray(v, np.float32).reshape(n, 128).T
    return out


def _prep_common(inp):
    vecs = np.zeros((128, NVEC, 8), np.float32)
    vecs[:, V_EV_NORM_MIX] = _feat(inp["ev_norm_mix"][0])
    vecs[:, V_EV_NORM_FFN] = _feat(inp["ev_norm_ffn"][0])
    vecs[:, V_XA_NORM0] = _feat(inp["xa_norm"][0])
    vecs[:, V_XA_NORMMEM0] = _feat(inp["xa_norm_mem"][0])
    vecs[:, V_XA_NORM1] = _feat(inp["xa_norm"][1])
    vecs[:, V_XA_NORMMEM1] = _feat(inp["xa_norm_mem"][1])
    vecs[:, V_OD_NORM_MIX] = _feat(inp["od_norm_mix"][0])
    vecs[:, V_PW1_BA] = _feat(inp["od_pw1_b"][0][:1024])
    vecs[:, V_PW1_BG] = _feat(inp["od_pw1_b"][0][1024:])
    vecs[:, V_DW_B] = _feat(inp["od_dw_b"][0])
    vecs[:, V_LN_G] = _feat(inp["od_ln_g"][0])
    vecs[:, V_LN_B] = _feat(inp["od_ln_b"][0])
    vecs[:, V_PW2_B] = _feat(inp["od_pw2_b"][0])
    vecs[:, V_NORM_MOE] = _feat(inp["od_norm_moe"][0])
    vecs[:, V_FINAL] = _feat(inp["final_norm"])
    vecs[:, V_POOL_SCALE] = _feat_small(inp["ev_pool_scale"][0], 4)
    for k in range(3):
        vecs[:, V_CONVA + k] = _feat_small(inp["ev_conv_a"][0][k], 4)
    for k in range(31):
        vecs[:, V_DW + k] = _feat(inp["od_dw_w"][0][k])
    perm_in = np.concatenate([np.concatenate([g * 512 + i * 128 + np.arange(128) for g in range(4)]) for i in range(4)])
    perm_pw1 = np.concatenate([np.concatenate([(2 * j) * 128 + np.arange(256), 1024 + (2 * j) * 128 + np.arange(256)]) for j in range(4)])
    f = lambda a: np.ascontiguousarray(np.asarray(a, np.float32))
    common = {
        "vecs": vecs,
        "ident": np.eye(128, dtype=np.float32),
        "xa_wq": f(inp["xa_wq"]), "xa_wkv": f(inp["xa_wkv"]), "xa_wo": f(inp["xa_wo"]),
        "w_in": f(np.asarray(inp["ev_w_in"][0])[:, perm_in]),
        "pool_w": f(inp["ev_pool_w"][0]),
        "w_out": f(inp["ev_w_out"][0]),
        "ffn_gu": f(inp["ev_ffn_gu"][0]),
        "ffn_down": f(inp["ev_ffn_down"][0]),
        "pw1": f(np.asarray(inp["od_pw1_w"][0])[:, perm_pw1]),
        "pw2": f(inp["od_pw2_w"][0]),
        "router": f(inp["od_router"][0]),
        "moe_gu": f(inp["od_moe_gu"][0]),
        "moe_down": f(inp["od_moe_down"][0]),
    }
    return common


def _prep_core(inp, core):
    b, half = core // 2, core % 2
    s0 = half * OWN
    x = np.asarray(inp["x"], np.float32)
    xT = np.zeros((D, N0), np.float32)
    mask = np.zeros((N0,), np.float32)
    g0, g1 = s0 - HALO, s0 + OWN + HALO
    v0, v1 = max(g0, 0), min(g1, SEQ)
    xT[:, v0 - g0:v1 - g0] = x[b, v0:v1, :].T
    mask[v0 - g0:v1 - g0] = 1.0
    memT = np.ascontiguousarray(np.asarray(inp["mem"], np.float32)[b].T)
    return {"xT": xT, "maskb": np.ascontiguousarray(np.broadcast_to(mask, (128, N0))), "memT": memT}


_L0_KEYS = ["xT", "maskb", "memT", "vecs", "ident", "xa_wq", "xa_wkv", "xa_wo", "w_in", "pool_w", "w_out", "ffn_gu", "ffn_down"]
_L1_KEYS = ["h1T", "maskb", "memT", "vecs", "ident", "xa_wq", "xa_wkv", "xa_wo", "pw1", "pw2", "router", "moe_gu", "moe_down"]
_FUSED_KEYS = ["xT", "maskb", "memT", "vecs", "ident", "xa_wq", "xa_wkv", "xa_wo", "w_in", "pool_w", "w_out", "ffn_gu", "ffn_down",
               "pw1", "pw2", "router", "moe_gu", "moe_down"]

_CACHE = {}
FUSED = True


def _get_prog(mode):
    if mode not in _CACHE:
        _CACHE[mode] = build_program(mode)[0]
    return _CACHE[mode]


def kernel(**inputs):
    common = _prep_common(inputs)
    cores = [dict(common, **_prep_core(inputs, i)) for i in range(8)]
    out = np.zeros((4, SEQ, D), np.float32)
    if FUSED:
        nc = _get_prog("FUSED")
        res = run_bass_kernel_spmd(nc, [{k: c[k] for k in _FUSED_KEYS} for c in cores], core_ids=list(range(8)))
        outs = [r["outT"] for r in res.results]
    else:
        nc0 = _get_prog("L0")
        res0 = run_bass_kernel_spmd(nc0, [{k: c[k] for k in _L0_KEYS} for c in cores], core_ids=list(range(8)))
        for i in range(8):
            cores[i]["h1T"] = np.asarray(res0.results[i]["h1o"])
        nc1 = _get_prog("L1")
        res1 = run_bass_kernel_spmd(nc1, [{k: c[k] for k in _L1_KEYS} for c in cores], core_ids=list(range(8)))
        outs = [r["outT"] for r in res1.results]
    for i in range(8):
        b, half = i // 2, i % 2
        out[b, half * OWN:(half + 1) * OWN, :] = np.asarray(outs[i]).T
    return out
```
